# Optimizing a Trainium2 kernel written in Bass

```python
import jax, jax.numpy as jnp
from jax import lax
import numpy as np

D_MODEL = 2048
BATCH = 8
SEQ = 4096
DEPTH = 4

DILATED_PAIRS = ((128, 1), (512, 4), (2048, 16))
N_A_GROUPS = len(DILATED_PAIRS)
A_HEAD_DIM = 128
A_WIDTH = D_MODEL // 4
A_HEADS_PER_GROUP = A_WIDTH // A_HEAD_DIM
A_HEADS = N_A_GROUPS * A_HEADS_PER_GROUP
GLA_HEADS = 4
GLA_DV_TOTAL = 3 * D_MODEL // 8
GLA_DK_TOTAL = GLA_DV_TOTAL // 2
GLA_DV = GLA_DV_TOTAL // GLA_HEADS
GLA_DK = GLA_DK_TOTAL // GLA_HEADS
GLA_GATE_RANK = 16
GLA_TAU = 16.0
GLA_CHUNK = 64
SGU_WIDTH = 3 * D_MODEL // 8
SGU_GROUPS = 4
SGU_CHUNK = 128
N_BRANCHES = 3
MIX_WIDTH = A_WIDTH + GLA_DV_TOTAL + SGU_WIDTH
D_FFN = ((8 * D_MODEL + 3 * 256 - 1) // (3 * 256)) * 256
REL_BUCKETS = 32
REL_MAX_DIST = 2048
EPS = 1e-6
IN_WIDTHS = (N_A_GROUPS * A_WIDTH, N_A_GROUPS * A_WIDTH, N_A_GROUPS * A_WIDTH,
             GLA_DK_TOTAL, GLA_DK_TOTAL, GLA_DV_TOTAL, GLA_GATE_RANK,
             SGU_WIDTH, SGU_WIDTH,
             D_MODEL, D_MODEL, D_MODEL)
D_IN = sum(IN_WIDTHS)

kernel_name = "hybrid_dilated_gla_sgu_gated_block"


def rms_norm(x, g):
    xf = x.astype(jnp.float32)
    y = xf * lax.rsqrt(jnp.mean(xf * xf, axis=-1, keepdims=True) + EPS)
    return (y * g.astype(jnp.float32)).astype(x.dtype)


def layer_norm(x, g, b):
    xf = x.astype(jnp.float32)
    mu = jnp.mean(xf, axis=-1, keepdims=True)
    xc = xf - mu
    y = xc * lax.rsqrt(jnp.mean(xc * xc, axis=-1, keepdims=True) + EPS)
    return (y * g.astype(jnp.float32) + b.astype(jnp.float32)).astype(x.dtype)


def t5_causal_bucket(dist):
    max_exact = REL_BUCKETS // 2
    d = np.maximum(dist, 1)
    large = max_exact + (np.log(d / max_exact) / np.log(REL_MAX_DIST / max_exact)
                         * (REL_BUCKETS - max_exact)).astype(np.int64)
    large = np.minimum(large, REL_BUCKETS - 1)
    return np.where(dist < max_exact, dist, large).astype(np.int32)


def dilated_window_attention(q, k, v, bias_heads, window, dilation):
    B, S, H, Dh = q.shape
    n_back = window // dilation
    blk = n_back
    L = S // dilation
    nb = -(-L // blk)
    Lp = nb * blk

    def to_sub(t):
        t = t.reshape(B, L, dilation, H, Dh).transpose(0, 2, 3, 1, 4)
        return jnp.pad(t, ((0, 0), (0, 0), (0, 0), (0, Lp - L), (0, 0)))

    def band(t):
        tp = jnp.pad(t, ((0, 0), (0, 0), (0, 0), (blk, 0), (0, 0))).reshape(B, dilation, H, nb + 1, blk, Dh)
        return jnp.concatenate([tp[:, :, :, :-1], tp[:, :, :, 1:]], axis=-2)

    qb = to_sub(q).reshape(B, dilation, H, nb, blk, Dh)
    kb = band(to_sub(k))
    vb = band(to_sub(v))

    i = np.arange(blk)[:, None]
    j = np.arange(2 * blk)[None, :]
    sub_dist = i + blk - j
    blk_id = np.arange(nb)[:, None, None]
    valid = (sub_dist >= 0) & (sub_dist <= n_back) & (blk_id * blk - blk + j >= 0)
    bucket = t5_causal_bucket(np.clip(sub_dist, 0, None) * dilation)
    bias = jnp.moveaxis(jnp.take(bias_heads, bucket, axis=0), -1, 0)

    logits = jnp.einsum('brhnqd,brhnkd->brhnqk', qb, kb) * (Dh ** -0.5) + bias[:, None]
    logits = jnp.where(valid, logits, -jnp.inf)
    m = jnp.max(logits, axis=-1, keepdims=True)
    p = jnp.exp(logits - m)
    s = jnp.sum(p, axis=-1, keepdims=True)
    o = jnp.einsum('brhnqk,brhnkd->brhnqd', p, vb) / s
    lse = (m + jnp.log(s))[..., 0]

    o = o.reshape(B, dilation, H, Lp, Dh)[:, :, :, :L].transpose(0, 3, 1, 2, 4).reshape(B, S, H, Dh)
    lse = lse.reshape(B, dilation, H, Lp)[..., :L].transpose(0, 3, 1, 2).reshape(B, S, H)
    return o, lse


def dilated_mixer(aq, ak, av, gq, gk, rel_bias):
    B, S, _ = aq.shape
    shp = (B, S, N_A_GROUPS, A_HEADS_PER_GROUP, A_HEAD_DIM)
    q = rms_norm(aq.reshape(shp), gq).astype(jnp.float32)
    k = rms_norm(ak.reshape(shp), gk).astype(jnp.float32)
    v = av.reshape(shp).astype(jnp.float32)
    outs, lses = [], []
    for gi, (window, dilation) in enumerate(DILATED_PAIRS):
        heads = slice(gi * A_HEADS_PER_GROUP, (gi + 1) * A_HEADS_PER_GROUP)
        o, lse = dilated_window_attention(q[:, :, gi], k[:, :, gi], v[:, :, gi],
                                          rel_bias[:, heads].astype(jnp.float32), window, dilation)
        outs.append(o)
        lses.append(lse)
    wts = jax.nn.softmax(jnp.stack(lses), axis=0)[..., None]
    o = jnp.sum(wts * jnp.stack(outs), axis=0)
    return o.reshape(B, S, A_WIDTH)


def gla_mixer(bq, bk, bv, b_gate_low, w_gate_up, b_gate, g_out):
    B, S, _ = bq.shape
    f32 = jnp.float32
    q = bq.reshape(B, S, GLA_HEADS, GLA_DK).astype(f32) * (GLA_DK ** -0.5)
    k = bk.reshape(B, S, GLA_HEADS, GLA_DK).astype(f32)
    v = bv.reshape(B, S, GLA_HEADS, GLA_DV).astype(f32)
    log_a = jax.nn.log_sigmoid((b_gate_low @ w_gate_up + b_gate).astype(f32)) / GLA_TAU
    log_a = log_a.reshape(B, S, GLA_HEADS, GLA_DK)
    n = S // GLA_CHUNK

    def chunks(t):
        return t.reshape(B, n, GLA_CHUNK, GLA_HEADS, t.shape[-1]).transpose(1, 0, 3, 2, 4)

    causal = np.tril(np.ones((GLA_CHUNK, GLA_CHUNK), dtype=bool))[:, :, None]

    def step(state, inp):
        qc, kc, vc, ac = inp
        b = jnp.cumsum(ac, axis=-2)
        inter = jnp.einsum('bhtk,bhkv->bhtv', qc * jnp.exp(b), state)
        diff = b[:, :, :, None, :] - b[:, :, None, :, :]
        decay = jnp.exp(jnp.where(causal, diff, -jnp.inf))
        scores = jnp.einsum('bhtk,bhsk,bhtsk->bhts', qc, kc, decay)
        intra = jnp.einsum('bhts,bhsv->bhtv', scores, vc)
        b_last = b[:, :, -1:, :]
        new_state = (jnp.exp(b_last[:, :, 0, :])[..., None] * state
                     + jnp.einsum('bhsk,bhsv->bhkv', kc * jnp.exp(b_last - b), vc))
        return new_state, inter + intra

    state0 = jnp.zeros((B, GLA_HEADS, GLA_DK, GLA_DV), f32)
    _, o = lax.scan(step, state0, (chunks(q), chunks(k), chunks(v), chunks(log_a)))
    o = o.transpose(1, 0, 3, 2, 4).reshape(B, S, GLA_HEADS, GLA_DV)
    o = rms_norm(o, g_out)
    return o.reshape(B, S, GLA_DV_TOTAL)


def sgu_mixer(cu, cv, ln_g, ln_b, w_s, b_s):
    B, S, _ = cu.shape
    u = jax.nn.gelu(cu)
    v = layer_norm(jax.nn.gelu(cv), ln_g, ln_b)
    n = S // SGU_CHUNK
    vg = v.reshape(B, n, SGU_CHUNK, SGU_GROUPS, SGU_WIDTH // SGU_GROUPS)
    mask = np.tril(np.ones((SGU_CHUNK, SGU_CHUNK), dtype=bool))
    w = jnp.where(mask, w_s, jnp.zeros_like(w_s))
    f = jnp.einsum('gts,bnsgc->bntgc', w, vg) + b_s.T[None, None, :, :, None]
    return u * f.reshape(B, S, SGU_WIDTH)


def setup_inputs(seed: int = 0) -> dict:
    key = jax.random.key(seed)
    ks = jax.random.split(key, 20)
    nrm = jax.random.normal
    f32 = jnp.float32
    x = nrm(ks[0], (BATCH, SEQ, D_MODEL), f32)
    rel_bias = 0.5 * nrm(ks[1], (REL_BUCKETS, A_HEADS), f32)
    norm1_g = 1.0 + 0.02 * nrm(ks[2], (DEPTH, D_MODEL), f32)
    w_in = nrm(ks[3], (DEPTH, D_MODEL, D_IN), f32) * D_MODEL ** -0.5
    q_norm_g = 1.0 + 0.02 * nrm(ks[4], (DEPTH, A_HEAD_DIM), f32)
    k_norm_g = 1.0 + 0.02 * nrm(ks[5], (DEPTH, A_HEAD_DIM), f32)
    gla_gate_up = nrm(ks[6], (DEPTH, GLA_GATE_RANK, GLA_DK_TOTAL), f32) * GLA_GATE_RANK ** -0.5
    gla_gate_b = 0.1 * nrm(ks[7], (DEPTH, GLA_DK_TOTAL), f32)
    gla_out_g = 1.0 + 0.02 * nrm(ks[8], (DEPTH, GLA_DV), f32)
    sgu_ln_g = 1.0 + 0.02 * nrm(ks[9], (DEPTH, SGU_WIDTH), f32)
    sgu_ln_b = 0.02 * nrm(ks[10], (DEPTH, SGU_WIDTH), f32)
    sgu_w = nrm(ks[11], (DEPTH, SGU_GROUPS, SGU_CHUNK, SGU_CHUNK), f32) * SGU_CHUNK ** -0.5
    sgu_b = 1.0 + 0.02 * nrm(ks[12], (DEPTH, SGU_GROUPS, SGU_CHUNK), f32)
    w_branch = jnp.concatenate([
        nrm(ks[13], (DEPTH, A_WIDTH, D_MODEL), f32) * A_WIDTH ** -0.5,
        nrm(ks[14], (DEPTH, GLA_DV_TOTAL, D_MODEL), f32) * GLA_DV_TOTAL ** -0.5,
        nrm(ks[15], (DEPTH, SGU_WIDTH, D_MODEL), f32) * SGU_WIDTH ** -0.5], axis=1)
    w_out = nrm(ks[16], (DEPTH, D_MODEL, D_MODEL), f32) * D_MODEL ** -0.5
    norm2_g = 1.0 + 0.02 * nrm(ks[17], (DEPTH, D_MODEL), f32)
    w_ffn_in = nrm(ks[18], (DEPTH, D_MODEL, 2 * D_FFN), f32) * D_MODEL ** -0.5
    w_ffn_out = nrm(ks[19], (DEPTH, D_FFN, D_MODEL), f32) * D_FFN ** -0.5
    return dict(x=x, rel_bias=rel_bias, norm1_g=norm1_g, w_in=w_in, q_norm_g=q_norm_g, k_norm_g=k_norm_g,
                gla_gate_up=gla_gate_up, gla_gate_b=gla_gate_b, gla_out_g=gla_out_g,
                sgu_ln_g=sgu_ln_g, sgu_ln_b=sgu_ln_b, sgu_w=sgu_w, sgu_b=sgu_b,
                w_branch=w_branch, w_out=w_out, norm2_g=norm2_g, w_ffn_in=w_ffn_in, w_ffn_out=w_ffn_out)


def reference(x, rel_bias, norm1_g, w_in, q_norm_g, k_norm_g, gla_gate_up, gla_gate_b, gla_out_g,
              sgu_ln_g, sgu_ln_b, sgu_w, sgu_b, w_branch, w_out, norm2_g, w_ffn_in, w_ffn_out):
    in_splits = [int(s) for s in np.cumsum(IN_WIDTHS)[:-1]]
    branch_splits = [A_WIDTH, A_WIDTH + GLA_DV_TOTAL]
    for l in range(DEPTH):
        h = rms_norm(x, norm1_g[l])
        (aq, ak, av, bq, bk, bv, b_gate_low, cu, cv,
         gate_a, gate_b, gate_c) = jnp.split(h @ w_in[l], in_splits, axis=-1)
        o_a = dilated_mixer(aq, ak, av, q_norm_g[l], k_norm_g[l], rel_bias).astype(x.dtype)
        o_b = gla_mixer(bq, bk, bv, b_gate_low, gla_gate_up[l], gla_gate_b[l], gla_out_g[l]).astype(x.dtype)
        o_c = sgu_mixer(cu, cv, sgu_ln_g[l], sgu_ln_b[l], sgu_w[l], sgu_b[l])
        p_a, p_b, p_c = jnp.split(w_branch[l], branch_splits, axis=0)
        y = (jax.nn.sigmoid(gate_a) * (o_a @ p_a)
             + jax.nn.sigmoid(gate_b) * (o_b @ p_b)
             + jax.nn.sigmoid(gate_c) * (o_c @ p_c))
        x = x + y @ w_out[l]
        h = rms_norm(x, norm2_g[l])
        f_gate, f_up = jnp.split(h @ w_ffn_in[l], 2, axis=-1)
        x = x + (jax.nn.silu(f_gate) * f_up) @ w_ffn_out[l]
    return x
```

```python
import numpy as np
import concourse.bass as bass
import concourse.mybir as mybir
from concourse.bass_utils import run_bass_kernel_spmd

F32 = mybir.dt.float32
BF16 = mybir.dt.bfloat16
AF = mybir.ActivationFunctionType
ALU = mybir.AluOpType
AX = mybir.AxisListType

D = 2048
KC = 16
D_IN = 13840
D_FFN = 5632
EPS = 1e-6
SAME_ENGINE_SYNC = True

ENGS = ("pe", "act", "dve", "pool", "sp")


class Buf:
    __slots__ = ("name", "writers", "readers", "sem", "kind", "phase")

    def __init__(self, name, kind="sb", phase=False):
        self.phase = phase
        self.name = name
        self.writers = {}
        self.readers = {}
        self.sem = None
        self.kind = kind


class T:
    __slots__ = ("ap", "buf")

    def __init__(self, ap, buf):
        self.ap = ap
        self.buf = buf

    def __getitem__(self, k):
        return T(self.ap[k], self.buf)

    def re(self, s, **kw):
        return T(self.ap.rearrange(s, **kw), self.buf)

    def bc(self, dt):
        return T(self.ap.bitcast(dt), self.buf)


class Op:
    __slots__ = ("eng", "idx", "fn", "waits", "is_dma", "sem", "signals", "sigval", "inc", "is_bar")

    def __init__(self, eng, idx, fn):
        self.eng = eng
        self.idx = idx
        self.fn = fn
        self.waits = []
        self.is_dma = False
        self.sem = None
        self.signals = False
        self.sigval = 0
        self.inc = 16
        self.is_bar = False


class Prog:
    def __init__(self, nc):
        self.nc = nc
        self.ops = {e: [] for e in ENGS}
        self.seen = {e: {f: -1 for f in ENGS} for e in ENGS}
        self.seen_dma = {e: {} for e in ENGS}
        self.dma_sems = []
        self.pool = []
        self.pool_ptr = 0
        self.bg_sems = set()

    def new_dma_sem(self, phase=False):
        if phase:
            if self.pool_ptr < len(self.pool):
                si = self.pool[self.pool_ptr]
                self.pool_ptr += 1
                return si
        h = self.nc.alloc_semaphore(name=f"dq{len(self.dma_sems)}")
        self.dma_sems.append([h, 0])
        si = len(self.dma_sems) - 1
        if phase:
            self.pool.append(si)
            self.pool_ptr += 1
        return si

    def reset_pool(self):
        self.pool_ptr = 0

    def op(self, eng, fn, reads=(), writes=(), dma_sem=None):
        reads = [t.buf if isinstance(t, T) else t for t in reads]
        writes = [t.buf if isinstance(t, T) else t for t in writes]
        o = Op(eng, len(self.ops[eng]), fn)
        seen = self.seen[eng]
        seen_d = self.seen_dma[eng]
        deps = []
        for b in reads:
            deps.extend(b.writers.values())
        for b in writes:
            if b.kind != "dram":
                deps.extend(b.writers.values())
            deps.extend(b.readers.values())
        for d in deps:
            if d is o:
                continue
            if d.is_dma:
                val = self.dma_sems[d.sem][1]
                if seen_d.get(d.sem, 0) >= val:
                    continue
                seen_d[d.sem] = val
                o.waits.append(("d", d.sem, val))
            else:
                if d.eng == eng and (eng == "pe" or not SAME_ENGINE_SYNC or eng == "sp"):
                    continue
                if seen[d.eng] >= d.idx:
                    continue
                seen[d.eng] = d.idx
                d.signals = True
                o.waits.append(("c", d))
        if dma_sem is not None:
            o.is_dma = True
            o.sem = dma_sem
            self.dma_sems[dma_sem][1] += 16
        self.ops[eng].append(o)
        key = ("d", o.sem) if o.is_dma else ("c", eng)
        for b in reads:
            b.readers[key] = o
        for b in writes:
            if b.kind == "dram" and not b.readers:
                b.writers[key] = o
            else:
                b.writers = {key: o}
                b.readers = {}
        return o

    def dma(self, q, out, in_, sem_t=None, xr=(), xw=()):
        if sem_t is not None:
            sb = sem_t if isinstance(sem_t, Buf) else sem_t.buf
        elif out.buf.kind == "sb":
            sb = out.buf
        else:
            assert in_.buf.kind == "sb", (out.buf.name, in_.buf.name)
            sb = in_.buf
        if sb.sem is None:
            sb.sem = self.new_dma_sem(sb.phase)
        oa, ia = out.ap, in_.ap
        return self.op(q, lambda e: e.dma_start(out=oa, in_=ia), reads=[in_] + list(xr),
                       writes=[out] + list(xw), dma_sem=sb.sem)

    def barrier(self):
        lasts = []
        for f in ("pe", "act", "dve", "pool"):
            for o_ in reversed(self.ops[f]):
                if not o_.is_dma and o_.fn is not None and not getattr(o_, "is_bar", False):
                    lasts.append(o_)
                    break
        for e in ENGS:
            o = Op(e, len(self.ops[e]), (lambda eng: eng.nop()))
            o.is_bar = True
            for d in lasts:
                if self.seen[e][d.eng] >= d.idx:
                    continue
                self.seen[e][d.eng] = d.idx
                d.signals = True
                o.waits.append(("c", d))
            for si, (h, c) in enumerate(self.dma_sems):
                if si in self.bg_sems:
                    continue
                if c > 0 and self.seen_dma[e].get(si, 0) < c:
                    self.seen_dma[e][si] = c
                    o.waits.append(("d", si, c))
            self.ops[e].append(o)

    def mm(self, out, pairs, extra_reads=()):
        oa = out.ap
        aps = [(l.ap, r.ap) for (l, r) in pairs]
        n = len(aps)

        def fn(e):
            ins = None
            for i, (l, r) in enumerate(aps):
                ins = e.matmul(oa, l, r, start=(i == 0), stop=(i == n - 1))
            return ins
        rd = [x for p in pairs for x in p] + list(extra_reads)
        return self.op("pe", fn, reads=rd, writes=[out])

    def tr(self, out, in_, ident):
        oa, ia, da = out.ap, in_.ap, ident.ap
        return self.op("pe", lambda e: e.transpose(oa, ia, da), reads=[in_, ident], writes=[out])

    def act(self, out, in_, func, bias=None, scale=None, accum=None, eng="act"):
        kw = {}
        rd = [in_]
        wr = [out]
        if bias is not None:
            if isinstance(bias, T):
                kw["bias"] = bias.ap
                rd.append(bias)
            else:
                kw["bias"] = bias
        if scale is not None:
            if isinstance(scale, T):
                kw["scale"] = scale.ap
                rd.append(scale)
            else:
                kw["scale"] = scale
        if accum is not None:
            kw["accum_out"] = accum.ap
            wr.append(accum)
        oa, ia = out.ap, in_.ap
        return self.op("act", lambda e: e.activation(out=oa, in_=ia, func=func, **kw), reads=rd, writes=wr)

    def tt(self, eng, out, in0, in1, op):
        oa, a, b = out.ap, in0.ap, in1.ap
        return self.op(eng, lambda e: e.tensor_tensor(out=oa, in0=a, in1=b, op=op), reads=[in0, in1], writes=[out])

    def ts(self, eng, out, in0, s1, s2, op0, op1=None):
        rd = [in0]
        a1 = s1
        a2 = s2
        if isinstance(s1, T):
            rd.append(s1)
            a1 = s1.ap
        if isinstance(s2, T):
            rd.append(s2)
            a2 = s2.ap
        oa, ia = out.ap, in0.ap
        if op1 is None:
            return self.op(eng, lambda e: e.tensor_scalar(out=oa, in0=ia, scalar1=a1, scalar2=None, op0=op0),
                           reads=rd, writes=[out])
        return self.op(eng, lambda e: e.tensor_scalar(out=oa, in0=ia, scalar1=a1, scalar2=a2, op0=op0, op1=op1),
                       reads=rd, writes=[out])

    def stt(self, eng, out, in0, scalar, in1, op0, op1):
        rd = [in0, in1]
        sa = scalar
        if isinstance(scalar, T):
            rd.append(scalar)
            sa = scalar.ap
        oa, a, b = out.ap, in0.ap, in1.ap
        return self.op(eng, lambda e: e.scalar_tensor_tensor(out=oa, in0=a, scalar=sa, in1=b, op0=op0, op1=op1),
                       reads=rd, writes=[out])

    def copy(self, eng, out, in_):
        oa, ia = out.ap, in_.ap
        if eng == "act":
            return self.op(eng, lambda e: e.activation(out=oa, in_=ia, func=AF.Copy), reads=[in_], writes=[out])
        return self.op(eng, lambda e: e.tensor_copy(out=oa, in_=ia), reads=[in_], writes=[out])

    def memset(self, eng, out, val):
        oa = out.ap
        return self.op(eng, lambda e: e.memset(oa, val), writes=[out])

    def emit(self):
        nc = self.nc
        csem = {}
        for e in ("pe", "act", "dve", "pool"):
            csem[e] = nc.alloc_semaphore(name=f"cs_{e}")
            c = 0
            for o in self.ops[e]:
                if o.signals:
                    c += 1
                    o.sigval = c
        dsem = self.dma_sems
        final_waits = [(h, c) for (h, c) in dsem if c > 0]

        def run(ename, eobj):
            for o in self.ops[ename]:
                for w in o.waits:
                    if w[0] == "d":
                        eobj.wait_ge(dsem[w[1]][0], w[2])
                    else:
                        d = w[1]
                        eobj.wait_ge(csem[d.eng], d.sigval)
                ins = o.fn(eobj)
                if o.is_dma:
                    ins.then_inc(dsem[o.sem][0], 16)
                elif o.signals:
                    ins.then_inc(csem[ename], 1)
            if ename == "sp":
                for (h, c) in final_waits:
                    eobj.wait_ge(h, c)

        with nc.Block() as block:
            @block.tensor
            def _(e):
                run("pe", e)

            @block.scalar
            def _(e):
                run("act", e)

            @block.vector
            def _(e):
                run("dve", e)

            @block.gpsimd
            def _(e):
                run("pool", e)

            @block.sync
            def _(e):
                run("sp", e)


DIL = (1, 4, 16)
REL_BUCKETS = 32
REL_MAX_DIST = 2048
ATT_SCALE = 128 ** -0.5
MASKNEG = -30000.0

WIN_BLOCKS = []
for _i in range(3):
    WIN_BLOCKS.append(("aq", 0 + 512 * _i, 512))
for _i in range(3):
    WIN_BLOCKS.append(("ak", 1536 + 512 * _i, 512))
for _i in range(3):
    WIN_BLOCKS.append(("av", 3072 + 512 * _i, 512))
WIN_BLOCKS.append(("gq", 4608, 384))
WIN_BLOCKS.append(("gk", 4992, 384))
WIN_BLOCKS.append(("gv", 5376, 384))
WIN_BLOCKS.append(("gv", 5760, 384))
WIN_BLOCKS.append(("glow", 6144, 16))
WIN_BLOCKS.append(("cu", 6160, 384))
WIN_BLOCKS.append(("cu", 6544, 384))
WIN_BLOCKS.append(("cv", 6928, 384))
WIN_BLOCKS.append(("cv", 7312, 384))
for _i in range(12):
    WIN_BLOCKS.append(("gate", 7696 + 512 * _i, 512))
N_WIN = len(WIN_BLOCKS)
BLK_BR = N_WIN
BLK_WO = BLK_BR + 8
BLK_F1 = BLK_WO + 4
BLK_F2 = BLK_F1 + 22
NBLK = BLK_F2 + 16
BLKE = 8192


def _t5_bucket(dist):
    max_exact = REL_BUCKETS // 2
    d = np.maximum(dist, 1)
    large = max_exact + (np.log(d / max_exact) / np.log(REL_MAX_DIST / max_exact)
                         * (REL_BUCKETS - max_exact)).astype(np.int64)
    large = np.minimum(large, REL_BUCKETS - 1)
    return np.where(dist < max_exact, dist, large).astype(np.int32)


def _bias_index_tables():
    j = np.arange(128)[:, None]
    i = np.arange(128)[None, :]
    idx = np.zeros((12, 2, 128, 128), np.int64)
    mask = np.zeros((2, 128, 128), np.float32)
    dprev = i + 128 - j
    dcur = i - j
    mask[1] = np.where(dprev <= 128, 0.0, MASKNEG)
    mask[0] = np.where(dcur >= 0, 0.0, MASKNEG)
    for hd in range(12):
        dil = DIL[hd // 4]
        idx[hd, 1] = _t5_bucket(np.clip(dprev, 0, None) * dil)
        idx[hd, 0] = _t5_bucket(np.clip(dcur, 0, None) * dil)
    return idx, mask


class Dram:
    def __init__(self, nc, name, shape, dt, kind="Internal", ntiles=1):
        self.ap = nc.dram_tensor(name, shape, dt, kind=kind).ap()
        self.bufs = [Buf(f"{name}_{i}", "dram") for i in range(ntiles)]

    def t(self, key, tile=0):
        return T(self.ap[key], self.bufs[tile])

    def all(self):
        return list(self.bufs)


def build(S=4096, depth=4, dbg=(), stop=None):
    nc = bass.Bass("TRN2", target_bir_lowering=False)
    P = Prog(nc)
    NTT = S // 512
    NT = S // 128

    def ext_in(name, shape, dt=F32):
        return Dram(nc, name, shape, dt, kind="ExternalInput")

    def scr(name, shape, dt=BF16, ntiles=NTT):
        kind = "ExternalOutput" if name in dbg else "Internal"
        return Dram(nc, name, shape, dt, kind=kind, ntiles=ntiles)

    x_in = ext_in("x", [S, D])
    abias_in = ext_in("abias", [128, 12, 2, 128])
    amask_in = ext_in("amask", [128, 2, 128])
    cst_in = ext_in("cst", [128, 4, 128])
    g1_in = ext_in("g1", [depth, 128, 16])
    g2_in = ext_in("g2", [depth, 128, 16])
    qkg_in = ext_in("qkg", [depth, 128, 2])
    gup_in = ext_in("gup", [depth, 16, 384])
    gb_in = ext_in("gb", [depth, 128, 384])
    gog_in = ext_in("gog", [depth, 96, 2])
    lng_in = ext_in("lng", [depth, 128, 768])
    lnb_in = ext_in("lnb", [depth, 128, 768])
    sw_in = ext_in("sw", [depth, 4, 128, 128])
    sb_in = ext_in("sbias", [depth, 1, 512])
    w_in = ext_in("w_in", [depth, D, D_IN])
    w_br = ext_in("w_branch", [depth, D, D])
    w_o = ext_in("w_out", [depth, D, D])
    w_f1 = ext_in("w_ffn_in", [depth, D, 2 * D_FFN])
    w_f2 = ext_in("w_ffn_out", [depth, D_FFN, D])
    y_out = Dram(nc, "y", [S, D], F32, kind="ExternalOutput", ntiles=NTT)

    xres = scr("xres", [128, 16, S], F32)
    wsc = [Dram(nc, f"wsc{p}", [NBLK, 128, BLKE], BF16, ntiles=NBLK) for p in range(2)]
    wsem = [[Buf(f"wsem{p}_{b}", "dram") for b in range((N_WIN + 4) if p == 0 else 8)] for p in range(2)]
    for p_ in range(2):
        for b_ in wsem[p_]:
            b_.sem = P.new_dma_sem()
            P.bg_sems.add(b_.sem)
    aq = scr("aq", [128, 12, S])
    ak = scr("ak", [128, 12, S])
    av = scr("av", [128, 12, S])
    gq = scr("gq", [96, 4, S])
    gk = scr("gk", [96, 4, S])
    gv = scr("gv", [96, 8, S])
    glow = scr("glow", [16, S])
    su = scr("su", [96, 8, S])
    vtok = scr("vtok", [NT, 128, 768])
    sg = scr("sg", [128, 48, S])
    oa = scr("oa", [128, 4, S])
    ob = scr("ob", [96, 8, S])
    oc = scr("oc", [96, 8, S])

    def sb(name, shape, dt):
        return T(nc.alloc_sbuf_tensor(name, shape, dt).ap(), Buf(name, "sb"))

    CST = sb("CST", [128, 4, 128], F32)
    IDB = sb("IDB", [128, 128], BF16)
    ONES = sb("ONES", [128, 128], BF16)
    TRIU = sb("TRIU", [128, 128], BF16)
    TRIR = sb("TRIR", [128, 128], BF16)
    ABT = sb("ABT", [128, 12, 2, 128], BF16)
    G1 = sb("G1", [128, 16], F32)
    G2 = sb("G2", [128, 16], F32)
    QKG = sb("QKG", [128, 2], F32)
    WUP = sb("WUP", [16, 384], BF16)
    GB = sb("GB", [128, 384], F32)
    GOG = sb("GOG", [96, 2], F32)
    LNG = sb("LNG", [128, 768], F32)
    LNB = sb("LNB", [128, 768], F32)
    WST = sb("WST", [128, 4, 128], BF16)
    BSH = sb("BSH", [1, 512], BF16)
    BSL = sb("BSL", [1, 512], BF16)

    ARENA_BYTES = nc.sbuf_bytes_remaining - 2048
    arena = nc.alloc_sbuf_tensor("arena", [128, ARENA_BYTES], mybir.dt.uint8).ap()

    class Arena:
        def __init__(self):
            self.off = 0
            P.reset_pool()

        def alloc(self, name, shape, dt):
            esz = 2 if dt == BF16 else 4
            n = int(np.prod(shape[1:])) * esz
            n = (n + 63) // 64 * 64
            assert self.off + n <= ARENA_BYTES, (name, self.off, n, ARENA_BYTES)
            ap = arena[:, self.off:self.off + n].bitcast(dt)
            ap = ap[:, 0:int(np.prod(shape[1:]))]
            if len(shape) == 3:
                ap = ap.rearrange("p (a b) -> p a b", a=shape[1])
            elif len(shape) == 4:
                ap = ap.rearrange("p (a b c) -> p a b c", a=shape[1], b=shape[2])
            ap = ap[0:shape[0]]
            self.off += n
            return T(ap, Buf(name, "sb", True))

    PSB = [T(nc.alloc_psum_tensor(f"ps{i}", [128, 512], F32).ap(), Buf(f"ps{i}", "ps")) for i in range(8)]

    pw = ALU.pow
    MUL = ALU.mult
    ADD = ALU.add

    def rsqrt_inplace(eng, t, in_, mulc):
        P.act(t, in_, AF.Ln, bias=EPS, scale=mulc)
        P.act(t, t, AF.Exp, scale=-0.5)

    P.dma("sp", CST, cst_in.t(slice(None)))
    P.copy("dve", IDB, CST[:, 0, :])
    P.copy("dve", TRIU, CST[:, 1, :])
    P.copy("dve", TRIR, CST[:, 2, :])
    P.memset("dve", ONES, 1.0)

    def wsem_idx(par, blk):
        if par == 0:
            return blk if blk < N_WIN else N_WIN + (blk % 4)
        return (blk % 4) if blk < N_WIN else 4 + (blk % 4)

    def convert(l):
        par = l % 2
        W = wsc[par]

        def cv(blk, out_ap, in_ap):
            P.dma("pool", T(out_ap, W.bufs[blk]), T(in_ap, Buf("wext", "dram")), sem_t=wsem[par][wsem_idx(par, blk)])
        for bi, (kind, c0, w) in enumerate(WIN_BLOCKS):
            cv(bi, W.ap[bi, :, 0:16 * w].rearrange("p (kc c) -> p kc c", kc=16),
               w_in.ap[l, :, c0:c0 + w].rearrange("(kc p) c -> p kc c", p=128))
        for b in range(8):
            c0 = b * 256
            blk = BLK_BR + b
            cv(blk, W.ap[blk, :, 0:1024].rearrange("p (kc c) -> p kc c", kc=4),
               w_br.ap[l, 0:512, c0:c0 + 256].rearrange("(kc p) c -> p kc c", p=128))
            cv(blk, W.ap[blk, 0:96, 1024:3072].rearrange("p (kc c) -> p kc c", kc=8),
               w_br.ap[l, 512:1280, c0:c0 + 256].rearrange("(kc p) c -> p kc c", p=96))
            cv(blk, W.ap[blk, 0:96, 3072:5120].rearrange("p (kc c) -> p kc c", kc=8),
               w_br.ap[l, 1280:2048, c0:c0 + 256].rearrange("(kc p) c -> p kc c", p=96))
        for b in range(4):
            blk = BLK_WO + b
            cv(blk, W.ap[blk, :, :].rearrange("p (kc c) -> p kc c", kc=16),
               w_o.ap[l, :, b * 512:(b + 1) * 512].rearrange("(kc p) c -> p kc c", p=128))
        for b in range(22):
            blk = BLK_F1 + b
            o4 = W.ap[blk, :, :].rearrange("p (kc t c) -> p kc t c", kc=16, t=2)
            cv(blk, o4[:, :, 0, :], w_f1.ap[l, :, b * 256:(b + 1) * 256].rearrange("(kc p) c -> p kc c", p=128))
            cv(blk, o4[:, :, 1, :],
               w_f1.ap[l, :, D_FFN + b * 256:D_FFN + (b + 1) * 256].rearrange("(kc p) c -> p kc c", p=128))
        for hf in range(2):
            for b in range(8):
                blk = BLK_F2 + hf * 8 + b
                cv(blk, W.ap[blk, :, 0:22 * 256].rearrange("p (kc c) -> p kc c", kc=22),
                   w_f2.ap[l, hf * 2816:(hf + 1) * 2816, b * 256:(b + 1) * 256].rearrange("(kc p) c -> p kc c", p=128))

    class WStream:
        def __init__(self, slots, seq, look=2):
            self.slots = slots
            self.seq = seq
            self.nxt = 0
            self.look = look

        def get(self, i):
            lim = min(i + self.look, len(self.seq) - 1)
            while self.nxt <= lim:
                par, blk, ne = self.seq[self.nxt]
                slot = self.slots[self.nxt % len(self.slots)]
                P.dma("sp", slot[:, 0:ne], wsc[par].t((blk, slice(None), slice(0, ne)), blk))
                self.nxt += 1
            return self.slots[i % len(self.slots)]

    def setup_x():
        A = Arena()
        XT = [A.alloc(f"XT{i}", [128, D], F32) for i in range(2)]
        XO = [A.alloc(f"XO{i}", [128, 16, 128], F32) for i in range(2)]
        for t in range(NT):
            xt = XT[t % 2]
            xo = XO[t % 2]
            P.dma("sp", xt, x_in.t((slice(t * 128, (t + 1) * 128), slice(None))))
            for q in range(4):
                ps = PSB[(t * 4 + q) % 8]
                for j in range(4):
                    kc = q * 4 + j
                    P.tr(ps[:, j * 128:(j + 1) * 128], xt[:, kc * 128:(kc + 1) * 128], CST[:, 0, :])
                P.copy("act" if q % 2 == 0 else "dve", xo[:, q * 4:(q + 1) * 4, :],
                       ps.re("p (a b) -> p a b", a=4))
            P.dma("sp", xres.t((slice(None), slice(None), slice(t * 128, (t + 1) * 128)), t // 4), xo)

    def load_params(l):
        A = Arena()
        P.dma("sp", G1, g1_in.t(l))
        P.dma("sp", G2, g2_in.t(l))
        P.dma("sp", QKG, qkg_in.t(l))
        WUP32 = A.alloc("WUP32", [16, 384], F32)
        P.dma("sp", WUP32, gup_in.t(l))
        P.copy("dve", WUP, WUP32)
        P.dma("sp", GB, gb_in.t(l))
        P.dma("sp", GOG, gog_in.t(l))
        P.dma("sp", LNG, lng_in.t(l))
        P.dma("sp", LNB, lnb_in.t(l))
        B32 = A.alloc("B32", [1, 512], F32)
        BH32 = A.alloc("BH32", [1, 512], F32)
        P.dma("sp", B32, sb_in.t(l))
        P.copy("dve", BSH, B32)
        P.copy("dve", BH32, BSH)
        P.tt("dve", BSL, B32, BH32, ALU.subtract)
        W32 = A.alloc("W32", [128, 4, 128], F32)
        WB = A.alloc("WB", [128, 4, 128], BF16)
        P.dma("sp", W32, T(sw_in.ap[l].rearrange("g t s -> t g s"), sw_in.bufs[0]))
        for g in range(4):
            P.tt("dve", WB[:, g, :], W32[:, g, :], CST[:, 3, :], MUL)
        psb = PSB[0].bc(BF16)
        for g in range(4):
            P.tr(psb[:, g * 128:(g + 1) * 128], WB[:, g, :], IDB)
        P.copy("dve", WST, psb[:, 0:512].re("p (g t) -> p g t", g=4))
        if l == 0:
            AB32 = A.alloc("AB32", [128, 12, 2, 128], F32)
            AM32 = A.alloc("AM32", [128, 2, 128], F32)
            P.dma("sp", AB32, abias_in.t(slice(None)))
            P.dma("sp", AM32, amask_in.t(slice(None)))
            for hd in range(12):
                P.stt("dve", ABT[:, hd], AB32[:, hd], float(128 ** 0.5), AM32, MUL, ADD)

    def norm_sq(XS, HT, kcs=range(16)):
        for kc in kcs:
            P.act(HT[:, kc, :], XS[:, kc, :], AF.Square)

    def norm_fin(XS, HT, G, RSTD, psx, SQ=None, use_pool=False):
        SQ = HT if SQ is None else SQ
        P.mm(psx, [(ONES, SQ[:, kc, :]) for kc in range(16)])
        rsqrt_inplace("dve", RSTD, psx, 1.0 / D)
        for kc in range(16):
            P.stt("pool" if (use_pool and kc % 2 == 1) else "dve", HT[:, kc, :], XS[:, kc, :], G[:, kc:kc + 1], RSTD, MUL, MUL)

    def rmsnorm_tile(XS, HT, G, RSTD, psx):
        norm_sq(XS, HT)
        norm_fin(XS, HT, G, RSTD, psx)

    def phase1(l):
        par = l % 2
        A = Arena()
        XSs = [A.alloc(f"XS{i}", [128, 16, 512], F32) for i in range(2)]
        HTs = [A.alloc(f"HT{i}", [128, 16, 512], BF16) for i in range(2)]
        WS = [A.alloc(f"WS{i}", [128, BLKE], BF16) for i in range(3)]
        RSTD = A.alloc("RSTD", [128, 512], F32)
        SQa = [A.alloc(f"SQa{i}", [128, 512], BF16) for i in range(2)]
        R = [A.alloc(f"R{i}", [128, 512], F32) for i in range(2)]
        OST = [A.alloc(f"OST{i}", [128, 512], BF16) for i in range(6)]
        CVG = A.alloc("CVG", [128, 4, 768], F32)
        TMP = [A.alloc(f"TMP{i}", [128, 768], F32) for i in range(2)]
        VTO = [A.alloc(f"VTO{i}", [128, 768], BF16) for i in range(2)]
        STT = A.alloc("STT", [128, 2, 6], F32)
        MV = A.alloc("MV", [128, 2], F32)
        RS = A.alloc("RS", [128, 1], F32)
        seq = []
        for tt in range(NTT):
            for bi, (kind, c0, w) in enumerate(WIN_BLOCKS):
                seq.append((par, bi, 16 * w))
        ws = WStream(WS, seq)
        ctr = {"ps": 0, "ost": 0, "aux": 0, "i": 0}

        def next_ps():
            ctr["ps"] += 1
            return PSB[ctr["ps"] % 4]

        def next_aux():
            ctr["aux"] += 1
            return PSB[4 + ctr["aux"] % 2]

        def next_ost():
            ctr["ost"] += 1
            return OST[ctr["ost"] % 6]

        pending = []

        def flush_pending():
            while pending:
                pending.pop(0)()

        P.dma("sp", XSs[0], xres.t((slice(None), slice(None), slice(0, 512)), 0))
        rmsnorm_tile(XSs[0], HTs[0], G1, RSTD, PSB[6])
        for tt in range(NTT):
            tok = slice(tt * 512, (tt + 1) * 512)
            XS, HT = XSs[tt % 2], HTs[tt % 2]
            XSn, HTn = XSs[(tt + 1) % 2], HTs[(tt + 1) % 2]
            for bi, (kind, c0, w) in enumerate(WIN_BLOCKS):
                if tt + 1 < NTT:
                    if bi == 0:
                        P.dma("sp", XSn, xres.t((slice(None), slice(None), slice((tt + 1) * 512, (tt + 2) * 512)), tt + 1))
                    elif bi == 6:
                        norm_sq(XSn, HTn)
                    elif bi == 14:
                        norm_fin(XSn, HTn, G1, RSTD, PSB[6])
                slot = ws.get(ctr["i"])
                ctr["i"] += 1
                sw = slot[:, 0:16 * w].re("p (kc c) -> p kc c", kc=16)
                if kind in ("aq", "ak", "av", "gate"):
                    for m in range(4):
                        ps = next_ps()
                        P.mm(ps, [(sw[:, kc, m * 128:(m + 1) * 128], HT[:, kc, :]) for kc in range(16)])
                        o = next_ost()
                        flush_pending()
                        if kind in ("aq", "ak"):
                            base = 0 if kind == "aq" else 1536
                            hd = (c0 - base) // 128 + m
                            qa = SQa[hd % 2]
                            r = R[hd % 2]
                            P.act(qa, ps, AF.Square)

                            def tail(kind=kind, hd=hd, qa=qa, r=r, ps=ps, o=o, tok=tok, tt=tt):
                                px = next_aux()
                                P.mm(px, [(ONES, qa)])
                                rsqrt_inplace("dve", r, px, 1.0 / 128)
                                P.stt("dve", o, ps, QKG[:, (0 if kind == "aq" else 1):(1 if kind == "aq" else 2)], r, MUL, MUL)
                                dst = (aq if kind == "aq" else ak)
                                P.dma("sp", dst.t((slice(None), hd, tok), tt), o)
                            pending.append(tail)
                        elif kind == "av":
                            hd = (c0 - 3072) // 128 + m
                            P.copy("act", o, ps)
                            P.dma("sp", av.t((slice(None), hd, tok), tt), o)
                        else:
                            gi = (c0 - 7696) // 128 + m
                            P.act(o, ps, AF.Sigmoid)
                            P.dma("sp", sg.t((slice(None), gi, tok), tt), o)
                elif kind in ("gq", "gk", "gv", "cu"):
                    for m in range(4):
                        ps = next_ps()
                        P.mm(ps[0:96], [(sw[:, kc, m * 96:(m + 1) * 96], HT[:, kc, :]) for kc in range(16)])
                        flush_pending()
                        o = next_ost()
                        if kind == "gq":
                            P.act(o[0:96], ps[0:96], AF.Copy, scale=float(96 ** -0.5))
                            P.dma("sp", gq.t((slice(None), m, tok), tt), o[0:96])
                        elif kind == "gk":
                            P.copy("act", o[0:96], ps[0:96])
                            P.dma("sp", gk.t((slice(None), m, tok), tt), o[0:96])
                        elif kind == "gv":
                            j = (c0 - 5376) // 96 + m
                            P.copy("act", o[0:96], ps[0:96])
                            P.dma("sp", gv.t((slice(None), j, tok), tt), o[0:96])
                        else:
                            j = (c0 - 6160) // 96 + m
                            P.act(o[0:96], ps[0:96], AF.Gelu)
                            P.dma("sp", su.t((slice(None), j, tok), tt), o[0:96])
                elif kind == "glow":
                    ps = next_ps()
                    P.mm(ps[0:16], [(sw[:, kc, 0:16], HT[:, kc, :]) for kc in range(16)])
                    o = next_ost()
                    P.copy("act", o[0:16], ps[0:16])
                    P.dma("sp", glow.t((slice(None), tok), tt), o[0:16])
                elif kind == "cv":
                    half = (c0 - 6928) // 384
                    for s in range(4):
                        ps = next_ps()
                        P.mm(ps[:, 0:384], [(HT[:, kc, s * 128:(s + 1) * 128], sw[:, kc, 0:384]) for kc in range(16)])
                        P.act(CVG[:, s, half * 384:(half + 1) * 384], ps[:, 0:384], AF.Gelu)
                    if half == 1:
                        for s in range(4):
                            tm = TMP[s % 2]
                            vo = VTO[s % 2]
                            c_ = CVG[:, s, :]
                            a0, a1 = STT[:, 0, :].ap, STT[:, 1, :].ap
                            i0, i1 = CVG[:, s, 0:384].ap, CVG[:, s, 384:768].ap
                            P.op("dve", lambda e, a0=a0, i0=i0: e.bn_stats(out=a0, in_=i0), reads=[CVG], writes=[STT])
                            P.op("dve", lambda e, a1=a1, i1=i1: e.bn_stats(out=a1, in_=i1), reads=[CVG], writes=[STT])
                            mva, sta = MV.ap, STT.re("p a b -> p (a b)").ap
                            P.op("dve", lambda e, mva=mva, sta=sta: e.bn_aggr(out=mva, in_=sta), reads=[STT], writes=[MV])
                            P.act(RS, MV[:, 1:2], AF.Ln, bias=EPS)
                            P.act(RS, RS, AF.Exp, scale=-0.5)
                            P.ts("dve", tm, c_, MV[:, 0:1], RS, ALU.subtract, MUL)
                            P.tt("dve", tm, tm, LNG, MUL)
                            P.tt("dve", vo, tm, LNB, ADD)
                            P.dma("sp", vtok.t(tt * 4 + s, tt), vo)

    def phase2_attn(l, A):
        QKV = [[A.alloc(f"QKV{i}{j}", [128, S], BF16) for j in range(3)] for i in range(2)]
        ACC = A.alloc("ACC", [128, 2, S], F32)
        ACCb = [Buf(f"ACC{c}", "sb", True) for c in range(NTT)]
        VT = [A.alloc(f"VT{i}", [128, 32, 128], BF16) for i in range(2)]
        PT = [A.alloc(f"PT{i}", [128, 2, 128], BF16) for i in range(4)]
        RC = [A.alloc(f"RC{i}", [128, 512], F32) for i in range(2)]
        OAS = [A.alloc(f"OAS{i}", [128, 512], BF16) for i in range(2)]
        cnt = 0
        pc = 0
        for hs in range(4):
            for c in range(NTT):
                for u_ in range(2):
                    ma = ACC[:, u_, c * 512:(c + 1) * 512].ap
                    P.op("pool", lambda e, ma=ma: e.memset(ma, 0.0), writes=[ACCb[c]])
            for g in range(3):
                hd = g * 4 + hs
                d = DIL[g]
                nb = (S // d) // 128
                Q, K, V = QKV[cnt % 2]
                vt = VT[cnt % 2]
                cnt += 1
                P.dma("sp", Q, T(aq.ap[:, hd, :], aq.bufs[0]), xr=aq.all())
                P.dma("sp", K, T(ak.ap[:, hd, :], ak.bufs[0]), xr=ak.all())
                P.dma("sp", V, T(av.ap[:, hd, :], av.bufs[0]), xr=av.all())

                def blk(r, n, d=d):
                    st = r + d * 128 * n
                    return slice(st, st + d * 127 + 1, d)
                if g == 2:
                    bl = [(r, n) for n in range(nb) for r in range(d)]
                else:
                    bl = [(r, n) for r in range(d) for n in range(nb)]
                bidx = {rn: i for i, rn in enumerate(bl)}
                for q4 in range(8):
                    psb = PSB[2 + q4 % 2].bc(BF16)
                    for j in range(4):
                        r, n = bl[q4 * 4 + j]
                        P.tr(psb[:, j * 128:(j + 1) * 128], V[:, blk(r, n)], IDB)
                    P.copy("act" if q4 % 2 == 0 else "dve", vt[:, q4 * 4:(q4 + 1) * 4, :],
                           psb[:, 0:512].re("p (a b) -> p a b", a=4))
                    yield

                def qsl_of(bi):
                    r, n = bl[bi]
                    w = 256 if n + 1 < nb else 128
                    st = r + d * 128 * n
                    return slice(st, st + d * (w - 1) + 1, d), w

                def stage_a(bi):
                    r, n = bl[bi]
                    qs, w = qsl_of(bi)
                    pss = PSB[bi % 2]
                    pt = PT[bi % 4].re("p a b -> p (a b)")
                    P.mm(pss[:, 0:w], [(K[:, blk(r, n)], Q[:, qs]),
                                       (IDB, ABT[:, hd].re("p a b -> p (a b)")[:, 0:w])])
                    P.act(pt[:, 0:w], pss[:, 0:w], AF.Exp, scale=ATT_SCALE)

                def stage_b(bi):
                    qs, w = qsl_of(bi)
                    pso = PSB[2 + bi % 2].re("p (a b) -> p a b", a=2)
                    pt = PT[bi % 4].re("p a b -> p (a b)")
                    P.mm(pso[:, 0, 0:w], [(vt[:, bi, :], pt[:, 0:w])])
                    P.mm(pso[:, 1, 0:w], [(ONES, pt[:, 0:w])])
                    r, n = bl[bi]
                    st = r + d * 128 * n
                    en = st + d * (w - 1)
                    cb = [ACCb[c] for c in range(st // 512, en // 512 + 1)]
                    aa, pa_ = ACC[:, :, qs].ap, pso[:, :, 0:w].ap
                    P.op("dve", lambda e, aa=aa, pa_=pa_: e.tensor_tensor(out=aa, in0=aa, in1=pa_, op=ADD),
                         reads=[pso] + cb, writes=cb)
                stage_a(0)
                stage_a(1)
                for bi in range(32):
                    if bi + 2 < 32:
                        stage_a(bi + 2)
                    stage_b(bi)
                    yield
            for c in range(NTT):
                tok = slice(c * 512, (c + 1) * 512)
                rc = RC[c % 2]
                o = OAS[c % 2]
                rca, ia = rc.ap, ACC[:, 1, tok].ap
                P.op("dve", lambda e, rca=rca, ia=ia: e.reciprocal(out=rca, in_=ia), reads=[ACCb[c]], writes=[rc])
                oa_, ua = o.ap, ACC[:, 0, tok].ap
                P.op("dve", lambda e, oa_=oa_, ua=ua, rca=rca: e.tensor_tensor(out=oa_, in0=ua, in1=rca, op=MUL),
                     reads=[ACCb[c], rc], writes=[o])
                P.dma("sp", oa.t((slice(None), hs, tok), c), o)
            yield

    def phase2_gla(l, A):
        GQ = [A.alloc(f"GQ{i}", [96, 4, 512], BF16) for i in range(1)] * 2
        GK = [A.alloc(f"GK{i}", [96, 4, 512], BF16) for i in range(1)] * 2
        GV = [A.alloc(f"GV{i}", [96, 8, 512], BF16) for i in range(1)] * 2
        GL = [A.alloc(f"GL{i}", [16, 512], BF16) for i in range(2)]
        PRE = A.alloc("PRE", [128, 384], F32)
        EE = A.alloc("EE", [128, 384], F32)
        LB = A.alloc("LB", [128, 384], BF16)
        EBP = [A.alloc(f"EBP{i}", [96, 4, 128], F32) for i in range(2)]
        EBN = A.alloc("EBN", [96, 4, 128], F32)
        ERB = A.alloc("ERB", [128, 384], F32)
        QT = [A.alloc(f"QT{i}", [96, 4, 128], BF16) for i in range(2)]
        KT = [A.alloc(f"KT{i}", [96, 4, 128], BF16) for i in range(2)]
        KH = [A.alloc(f"KH{i}", [128, 384], BF16) for i in range(2)]
        VTK = [A.alloc(f"VTK{i}", [128, 768], BF16) for i in range(2)]
        STM = [A.alloc(f"STM{i}", [128, 128], BF16) for i in range(4)]
        SF = [A.alloc(f"SF{h}", [96, 192], F32) for h in range(4)]
        SBF = [[A.alloc(f"SBF{h}{i}", [96, 192], BF16) for i in range(2)] for h in range(4)]
        SQG = A.alloc("SQG", [96, 8, 128], BF16)
        RN = A.alloc("RN", [96, 512], F32)
        OBS = [A.alloc(f"OBS{i}", [96, 8, 512], BF16) for i in range(1)] * 2
        B4, B5, B6, B7 = PSB[4], PSB[5], PSB[6], PSB[7]
        for c in range(NT):
            tt, s4 = c // 4, c % 4
            tok = slice(tt * 512, (tt + 1) * 512)
            sl = slice(s4 * 128, (s4 + 1) * 128)
            q_, k_, v_, gl_ = GQ[tt % 2], GK[tt % 2], GV[tt % 2], GL[tt % 2]
            obs = OBS[tt % 2]
            if s4 == 0:
                P.dma("sp", q_, gq.t((slice(None), slice(None), tok), tt))
                P.dma("sp", k_, gk.t((slice(None), slice(None), tok), tt))
                P.dma("sp", v_, gv.t((slice(None), slice(None), tok), tt))
                P.dma("sp", gl_, glow.t((slice(None), tok), tt))
            ebp, qt, kt, kh, vtk = EBP[c % 2], QT[c % 2], KT[c % 2], KH[c % 2], VTK[c % 2]
            P.mm(B4[:, 0:384], [(gl_[:, sl], WUP)])
            P.tt("dve", PRE, B4[:, 0:384], GB, ADD)
            P.act(EE, PRE, AF.Exp, scale=-1.0)
            P.act(LB, EE, AF.Ln, bias=1.0)
            psbt = B5.re("p (a b) -> p a b", a=4)
            for h in range(4):
                P.mm(psbt[0:96, h, :], [(LB[:, h * 96:(h + 1) * 96], TRIU)])
            P.mm(B4[:, 0:384], [(TRIR, LB)])
            P.act(ebp, psbt[0:96], AF.Exp, scale=-1.0 / 16)
            P.act(EBN, psbt[0:96], AF.Exp, scale=1.0 / 16)
            P.act(ERB, B4[:, 0:384], AF.Exp, scale=-1.0 / 16)
            P.tt("dve", qt, q_[:, :, sl], ebp, MUL)
            P.tt("dve", kt, k_[:, :, sl], EBN, MUL)
            pk = B5.bc(BF16)
            for h in range(4):
                P.tr(pk[:, h * 96:(h + 1) * 96], k_[:, h, sl], IDB[0:96, 0:96])
            pv = B6.bc(BF16)
            for j in range(8):
                P.tr(pv[:, j * 96:(j + 1) * 96], v_[:, j, sl], IDB[0:96, 0:96])
            P.tt("dve", kh, pk[:, 0:384], ERB, MUL)
            P.copy("act", vtk, pv[:, 0:768])
            yield
            pso = [B6.re("p (a b) -> p a b", a=4), B7.re("p (a b) -> p a b", a=4)]
            for h in range(4):
                stm = STM[h]
                P.mm(B4[:, 0:128], [(kt[:, h, :], qt[:, h, :])])
                P.tt("dve", stm, B4[:, 0:128], TRIU, MUL)
                sbf_old = SBF[h][(c + 1) % 2]
                sbf_new = SBF[h][c % 2]
                for j in range(2):
                    prs = [(vtk[:, h * 192 + j * 96:h * 192 + (j + 1) * 96], stm)]
                    if c > 0:
                        prs.append((sbf_old[:, j * 96:(j + 1) * 96], qt[:, h, :]))
                    P.mm(pso[h // 2][0:96, (h % 2) * 2 + j, :], prs)
                P.mm(B5[0:96, 192:384], [(kh[:, h * 96:(h + 1) * 96], vtk[:, h * 192:(h + 1) * 192])])
                if c == 0:
                    P.copy("dve", SF[h], B5[0:96, 192:384])
                else:
                    P.stt("dve", SF[h], SF[h], ebp[:, h, 127:128], B5[0:96, 192:384], MUL, ADD)
                if c < NT - 1:
                    P.copy("act", sbf_new, SF[h])
                if h == 1:
                    yield
            P.act(SQG[:, 0:4, :], pso[0][0:96], AF.Square)
            P.act(SQG[:, 4:8, :], pso[1][0:96], AF.Square)
            for h in range(4):
                P.mm(B4[0:96, h * 128:(h + 1) * 128],
                     [(ONES[0:96, 0:96], SQG[:, 2 * h, :]), (ONES[0:96, 0:96], SQG[:, 2 * h + 1, :])])
            rsqrt_inplace("dve", RN, B4[0:96, :], 1.0 / 192)
            for h in range(4):
                for j in range(2):
                    P.stt("dve", obs[:, 2 * h + j, sl], pso[h // 2][0:96, (h % 2) * 2 + j, :], GOG[:, j:j + 1],
                          RN[:, h * 128:(h + 1) * 128], MUL, MUL)
            if s4 == 3:
                P.dma("sp", ob.t((slice(None), slice(None), tok), tt), obs)
            yield

    def phase2_sgu(l, A):
        SU = [A.alloc(f"SU{i}", [96, 8, 512], BF16) for i in range(1)] * 2
        VK = [A.alloc(f"VK{i}", [128, 768], BF16) for i in range(3)]
        OCS = [A.alloc(f"OCS{i}", [96, 8, 512], BF16) for i in range(1)] * 2
        for c in range(NT):
            tt, s4 = c // 4, c % 4
            tok = slice(tt * 512, (tt + 1) * 512)
            sl = slice(s4 * 128, (s4 + 1) * 128)
            su_, ocs, vk = SU[tt % 2], OCS[tt % 2], VK[c % 3]
            if s4 == 0:
                P.dma("sp", su_, su.t((slice(None), slice(None), tok), tt))
            P.dma("sp", vk, vtok.t(c, tt))
            for hf in range(2):
                psf = PSB[hf].re("p (a b) -> p a b", a=4)
                for jj in range(4):
                    j = hf * 4 + jj
                    g = j // 2
                    P.mm(psf[0:96, jj, :], [(vk[:, j * 96:(j + 1) * 96], WST[:, g, :]),
                                            (ONES[0:1, 0:96], BSH[0:1, g * 128:(g + 1) * 128]),
                                            (ONES[0:1, 0:96], BSL[0:1, g * 128:(g + 1) * 128])])
                P.tt("dve", ocs[:, hf * 4:(hf + 1) * 4, sl], psf[0:96], su_[:, hf * 4:(hf + 1) * 4, sl], MUL)
            if s4 == 3:
                P.dma("sp", oc.t((slice(None), slice(None), tok), tt), ocs)
            yield

    def phase2(l):
        A = Arena()
        ga = phase2_attn(l, A)
        gg = phase2_gla(l, A)
        gs = phase2_sgu(l, A)
        live = {"a": ga, "g": gg, "s": gs}

        def step(k, n=1):
            for _ in range(n):
                if k in live:
                    try:
                        next(live[k])
                    except StopIteration:
                        del live[k]
        while live:
            step("a", 5)
            step("g", 1)
            step("s", 1 if "g" not in live or True else 0)
            step("a", 5)
            step("g", 1)
            step("a", 5)
            step("g", 1)

    def phase3(l, last):
        par = l % 2
        A = Arena()
        XS = A.alloc("XS", [128, 16, 512], F32)
        HT = A.alloc("HT", [128, 16, 512], BF16)
        WS = [A.alloc(f"WS{i}", [128, BLKE], BF16) for i in range(3)]
        RSTD = A.alloc("RSTD", [128, 512], F32)
        OA = A.alloc("OA", [128, 4, 512], BF16)
        OB = A.alloc("OB", [96, 8, 512], BF16)
        OC = A.alloc("OC", [96, 8, 512], BF16)
        SGT = [A.alloc(f"SGT{i}", [128, 3, 512], BF16) for i in range(3)]
        T1 = [A.alloc(f"T1{i}", [128, 512], F32) for i in range(2)]
        T2 = [A.alloc(f"T2{i}", [128, 512], F32) for i in range(2)]
        T3 = [A.alloc(f"T3{i}", [128, 512], F32) for i in range(2)]
        SIL = [A.alloc(f"SIL{i}", [128, 512], F32) for i in range(2)]
        ACTB = A.alloc("ACTB", [128, 22, 512], BF16)
        SQ3 = A.alloc("SQ3", [128, 16, 512], BF16)
        if last:
            yv = ACTB.re("p a b -> p (a b)").bc(F32)
            YO = [yv[:, 0:D], yv[:, D:2 * D]]
        else:
            YO = None
        seq = []
        for tt in range(NTT):
            seq += [(par, BLK_BR + b, 5120) for b in range(8)]
            seq += [(par, BLK_WO + b, 8192) for b in range(4)]
            for hf in range(2):
                seq += [(par, BLK_F1 + hf * 11 + b, 8192) for b in range(11)]
                seq += [(par, BLK_F2 + hf * 8 + b, 22 * 256) for b in range(8)]
        ws = WStream(WS, seq)
        wi = 0
        pc = 0
        sg4 = sg.ap.rearrange("p (b m) s -> p b m s", b=3)
        sgs = {"n": 0}

        def sgt_prefetch(upto):
            while sgs["n"] <= min(upto, NTT * 16 - 1):
                i = sgs["n"]
                t_, m_ = i // 16, i % 16
                P.dma("act", SGT[i % 3], T(sg4[:, :, m_, t_ * 512:(t_ + 1) * 512], sg.bufs[t_]))
                sgs["n"] += 1
        for tt in range(NTT):
            tok = slice(tt * 512, (tt + 1) * 512)
            if tt == 0:
                P.dma("act", OA, oa.t((slice(None), slice(None), tok), tt))
                P.dma("act", OB, ob.t((slice(None), slice(None), tok), tt))
                P.dma("act", OC, oc.t((slice(None), slice(None), tok), tt))
            for b in range(8):
                slot = ws.get(wi)
                wi += 1
                wa = slot[:, 0:1024].re("p (kc c) -> p kc c", kc=4)
                wb = slot[0:96, 1024:3072].re("p (kc c) -> p kc c", kc=8)
                wc = slot[0:96, 3072:5120].re("p (kc c) -> p kc c", kc=8)
                for mm_ in range(2):
                    m = b * 2 + mm_
                    cs = slice(mm_ * 128, (mm_ + 1) * 128)
                    sgt_prefetch(tt * 16 + m + 2)
                    sgt = SGT[(tt * 16 + m) % 3]
                    pa, pb, pcx = PSB[pc % 8], PSB[(pc + 1) % 8], PSB[(pc + 2) % 8]
                    pc += 3
                    P.mm(pa, [(wa[:, kc, cs], OA[:, kc, :]) for kc in range(4)])
                    P.mm(pb, [(wb[:, kc, cs], OB[:, kc, :]) for kc in range(8)])
                    P.mm(pcx, [(wc[:, kc, cs], OC[:, kc, :]) for kc in range(8)])
                    t1, t2, t3 = T1[m % 2], T2[m % 2], T3[m % 2]
                    P.tt("dve", t1, pa, sgt[:, 0, :], MUL)
                    P.tt("dve", t2, pb, sgt[:, 1, :], MUL)
                    P.tt("dve", t3, pcx, sgt[:, 2, :], MUL)
                    P.tt("pool", t1, t1, t2, ADD)
                    P.tt("pool", HT[:, m, :], t1, t3, ADD)
            P.dma("act", XS, xres.t((slice(None), slice(None), tok), tt))
            if tt + 1 < NTT:
                tokn = slice((tt + 1) * 512, (tt + 2) * 512)
                P.dma("act", OA, oa.t((slice(None), slice(None), tokn), tt + 1))
                P.dma("act", OB, ob.t((slice(None), slice(None), tokn), tt + 1))
                P.dma("act", OC, oc.t((slice(None), slice(None), tokn), tt + 1))
            for b in range(4):
                slot = ws.get(wi)
                wi += 1
                sw = slot.re("p (kc c) -> p kc c", kc=16)
                for mm_ in range(4):
                    m = b * 4 + mm_
                    ps = PSB[pc % 8]
                    pc += 1
                    P.mm(ps, [(sw[:, kc, mm_ * 128:(mm_ + 1) * 128], HT[:, kc, :]) for kc in range(16)])
                    P.tt("dve", XS[:, m, :], XS[:, m, :], ps, ADD)
                    P.act(SQ3[:, m, :], XS[:, m, :], AF.Square)
            norm_fin(XS, HT, G2, RSTD, PSB[pc % 8], SQ=SQ3, use_pool=False)
            pc += 1
            for hf in range(2):
                for b in range(11):
                    slot = ws.get(wi)
                    wi += 1
                    sw = slot.re("p (kc t c) -> p kc t c", kc=16, t=2)
                    for mm_ in range(2):
                        j = b * 2 + mm_
                        cs = slice(mm_ * 128, (mm_ + 1) * 128)
                        pg, pu = PSB[pc % 8], PSB[(pc + 1) % 8]
                        pc += 2
                        P.mm(pg, [(sw[:, kc, 0, cs], HT[:, kc, :]) for kc in range(16)])
                        P.mm(pu, [(sw[:, kc, 1, cs], HT[:, kc, :]) for kc in range(16)])
                        sil = SIL[j % 2]
                        P.act(sil, pg, AF.Silu)
                        P.tt("dve", ACTB[:, j, :], sil, pu, MUL)
                for b in range(8):
                    slot = ws.get(wi)
                    wi += 1
                    sw = slot[:, 0:22 * 256].re("p (kc c) -> p kc c", kc=22)
                    for mm_ in range(2):
                        m = b * 2 + mm_
                        ps = PSB[pc % 8]
                        pc += 1
                        P.mm(ps, [(sw[:, kc, mm_ * 128:(mm_ + 1) * 128], ACTB[:, kc, :]) for kc in range(22)])
                        P.tt("dve", XS[:, m, :], XS[:, m, :], ps, ADD)
            if not last:
                P.dma("act", xres.t((slice(None), slice(None), tok), tt), XS)
            else:
                for s in range(4):
                    yo = YO[s % 2]
                    for q in range(4):
                        ps = PSB[pc % 8]
                        pc += 1
                        for j in range(4):
                            kc = q * 4 + j
                            P.tr(ps[:, j * 128:(j + 1) * 128], XS[:, kc, s * 128:(s + 1) * 128], CST[:, 0, :])
                        P.copy("act" if q % 2 == 0 else "dve", yo[:, q * 512:(q + 1) * 512], ps)
                    r0 = tt * 512 + s * 128
                    P.dma("act", y_out.t((slice(r0, r0 + 128), slice(None)), tt), yo)

    order = ["setup", "convert", "params", "p1", "p2a", "p2b", "p2c", "p3"]
    lim = order.index(stop) if stop else len(order)

    def prog():
        setup_x()
        if lim < 1:
            return
        convert(0)
        P.barrier()
        for l in range(depth):
            if lim < 2:
                return
            load_params(l)
            P.barrier()
            if l + 1 < depth:
                convert(l + 1)
            if lim < 3:
                return
            phase1(l)
            P.barrier()
            if lim < 4:
                return
            phase2(l)
            P.barrier()
            if lim < 7:
                return
            phase3(l, l == depth - 1)
            P.barrier()
    prog()
    build.nsems = len(P.dma_sems)
    P.emit()
    return nc


def _consts():
    i = np.arange(128)
    c = np.zeros((128, 4, 128), np.float32)
    c[:, 0, :] = np.eye(128, dtype=np.float32)
    c[:, 1, :] = (i[:, None] <= i[None, :])
    c[:, 2, :] = (i[:, None] > i[None, :])
    c[:, 3, :] = (i[:, None] >= i[None, :])
    return c


def prep_shared(inp, depth):
    f = np.float32
    idx, mask = _bias_index_tables()
    rb = np.asarray(inp["rel_bias"], f)
    ab = np.zeros((12, 2, 128, 128), f)
    for hd in range(12):
        ab[hd] = rb[idx[hd], hd]
    sh = {}
    sh["abias"] = np.ascontiguousarray(ab.transpose(2, 0, 1, 3))
    sh["amask"] = np.ascontiguousarray(mask.transpose(1, 0, 2))
    sh["cst"] = _consts()
    sh["g1"] = np.ascontiguousarray(np.asarray(inp["norm1_g"], f)[:depth].reshape(depth, 16, 128).transpose(0, 2, 1))
    sh["g2"] = np.ascontiguousarray(np.asarray(inp["norm2_g"], f)[:depth].reshape(depth, 16, 128).transpose(0, 2, 1))
    sh["qkg"] = np.ascontiguousarray(np.stack([np.asarray(inp["q_norm_g"], f)[:depth],
                                               np.asarray(inp["k_norm_g"], f)[:depth]], axis=-1))
    sh["gup"] = np.ascontiguousarray(np.asarray(inp["gla_gate_up"], f)[:depth])
    sh["gb"] = np.ascontiguousarray(np.broadcast_to(np.asarray(inp["gla_gate_b"], f)[:depth, None, :], (depth, 128, 384)))
    sh["gog"] = np.ascontiguousarray(np.asarray(inp["gla_out_g"], f)[:depth].reshape(depth, 2, 96).transpose(0, 2, 1))
    sh["lng"] = np.ascontiguousarray(np.broadcast_to(np.asarray(inp["sgu_ln_g"], f)[:depth, None, :], (depth, 128, 768)))
    sh["lnb"] = np.ascontiguousarray(np.broadcast_to(np.asarray(inp["sgu_ln_b"], f)[:depth, None, :], (depth, 128, 768)))
    sh["sw"] = np.ascontiguousarray(np.asarray(inp["sgu_w"], f)[:depth])
    sh["sbias"] = np.ascontiguousarray(np.asarray(inp["sgu_b"], f)[:depth].reshape(depth, 1, 512))
    sh["w_in"] = np.ascontiguousarray(np.asarray(inp["w_in"], f)[:depth])
    sh["w_branch"] = np.ascontiguousarray(np.asarray(inp["w_branch"], f)[:depth])
    sh["w_out"] = np.ascontiguousarray(np.asarray(inp["w_out"], f)[:depth])
    sh["w_ffn_in"] = np.ascontiguousarray(np.asarray(inp["w_ffn_in"], f)[:depth])
    sh["w_ffn_out"] = np.ascontiguousarray(np.asarray(inp["w_ffn_out"], f)[:depth])
    return sh


_NC_CACHE = {}


def run(inp, depth=4, n_cores=8, dbg=(), trace=False, stop=None):
    x = np.asarray(inp["x"], np.float32)
    B, S, _ = x.shape
    assert B == n_cores
    key = (S, depth, tuple(dbg), stop)
    if key not in _NC_CACHE:
        _NC_CACHE[key] = build(S=S, depth=depth, dbg=dbg, stop=stop)
    nc = _NC_CACHE[key]
    sh = prep_shared(inp, depth)
    in_maps = []
    for c in range(n_cores):
        m = dict(sh)
        m["x"] = np.ascontiguousarray(x[c])
        in_maps.append(m)
    res = run_bass_kernel_spmd(nc, in_maps, core_ids=list(range(n_cores)), trace=trace)
    return res


def kernel(**inputs):
    res = run(inputs, depth=4, n_cores=8)
    return np.stack([r["y"] for r in res.results], axis=0).astype(np.float32)
```

```python
import numpy as np
import concourse.bass as bass
import concourse.mybir as mybir
from concourse.bass_utils import run_bass_kernel_spmd

F32 = mybir.dt.float32
BF16 = mybir.dt.bfloat16
AF = mybir.ActivationFunctionType
ALU = mybir.AluOpType
AX = mybir.AxisListType

D = 2048
KC = 16
D_IN = 13840
D_FFN = 5632
EPS = 1e-6
SAME_ENGINE_SYNC = True

ENGS = ("pe", "act", "dve", "pool", "sp")


class Buf:
    __slots__ = ("name", "writers", "readers", "sem", "kind", "phase")

    def __init__(self, name, kind="sb", phase=False):
        self.phase = phase
        self.name = name
        self.writers = {}
        self.readers = {}
        self.sem = None
        self.kind = kind


class T:
    __slots__ = ("ap", "buf")

    def __init__(self, ap, buf):
        self.ap = ap
        self.buf = buf

    def __getitem__(self, k):
        return T(self.ap[k], self.buf)

    def re(self, s, **kw):
        return T(self.ap.rearrange(s, **kw), self.buf)

    def bc(self, dt):
        return T(self.ap.bitcast(dt), self.buf)


class Op:
    __slots__ = ("eng", "idx", "fn", "waits", "is_dma", "sem", "signals", "sigval", "inc", "is_bar")

    def __init__(self, eng, idx, fn):
        self.eng = eng
        self.idx = idx
        self.fn = fn
        self.waits = []
        self.is_dma = False
        self.sem = None
        self.signals = False
        self.sigval = 0
        self.inc = 16
        self.is_bar = False


class Prog:
    def __init__(self, nc):
        self.nc = nc
        self.ops = {e: [] for e in ENGS}
        self.seen = {e: {f: -1 for f in ENGS} for e in ENGS}
        self.seen_dma = {e: {} for e in ENGS}
        self.dma_sems = []
        self.pool = []
        self.pool_ptr = 0
        self.bg_sems = set()

    def new_dma_sem(self, phase=False):
        if phase:
            if self.pool_ptr < len(self.pool):
                si = self.pool[self.pool_ptr]
                self.pool_ptr += 1
                return si
        h = self.nc.alloc_semaphore(name=f"dq{len(self.dma_sems)}")
        self.dma_sems.append([h, 0])
        si = len(self.dma_sems) - 1
        if phase:
            self.pool.append(si)
            self.pool_ptr += 1
        return si

    def reset_pool(self):
        self.pool_ptr = 0

    def op(self, eng, fn, reads=(), writes=(), dma_sem=None):
        reads = [t.buf if isinstance(t, T) else t for t in reads]
        writes = [t.buf if isinstance(t, T) else t for t in writes]
        o = Op(eng, len(self.ops[eng]), fn)
        seen = self.seen[eng]
        seen_d = self.seen_dma[eng]
        deps = []
        for b in reads:
            deps.extend(b.writers.values())
        for b in writes:
            if b.kind != "dram":
                deps.extend(b.writers.values())
            deps.extend(b.readers.values())
        for d in deps:
            if d is o:
                continue
            if d.is_dma:
                val = self.dma_sems[d.sem][1]
                if seen_d.get(d.sem, 0) >= val:
                    continue
                seen_d[d.sem] = val
                o.waits.append(("d", d.sem, val))
            else:
                if d.eng == eng and (eng == "pe" or not SAME_ENGINE_SYNC or eng == "sp"):
                    continue
                if seen[d.eng] >= d.idx:
                    continue
                seen[d.eng] = d.idx
                d.signals = True
                o.waits.append(("c", d))
        if dma_sem is not None:
            o.is_dma = True
            o.sem = dma_sem
            self.dma_sems[dma_sem][1] += 16
        self.ops[eng].append(o)
        key = ("d", o.sem) if o.is_dma else ("c", eng)
        for b in reads:
            b.readers[key] = o
        for b in writes:
            if b.kind == "dram" and not b.readers:
                b.writers[key] = o
            else:
                b.writers = {key: o}
                b.readers = {}
        return o

    def dma(self, q, out, in_, sem_t=None, xr=(), xw=()):
        if sem_t is not None:
            sb = sem_t if isinstance(sem_t, Buf) else sem_t.buf
        elif out.buf.kind == "sb":
            sb = out.buf
        else:
            assert in_.buf.kind == "sb", (out.buf.name, in_.buf.name)
            sb = in_.buf
        if sb.sem is None:
            sb.sem = self.new_dma_sem(sb.phase)
        oa, ia = out.ap, in_.ap
        return self.op(q, lambda e: e.dma_start(out=oa, in_=ia), reads=[in_] + list(xr),
                       writes=[out] + list(xw), dma_sem=sb.sem)

    def barrier(self):
        lasts = []
        for f in ("pe", "act", "dve", "pool"):
            for o_ in reversed(self.ops[f]):
                if not o_.is_dma and o_.fn is not None and not getattr(o_, "is_bar", False):
                    lasts.append(o_)
                    break
        for e in ENGS:
            o = Op(e, len(self.ops[e]), (lambda eng: eng.nop()))
            o.is_bar = True
            for d in lasts:
                if self.seen[e][d.eng] >= d.idx:
                    continue
                self.seen[e][d.eng] = d.idx
                d.signals = True
                o.waits.append(("c", d))
            for si, (h, c) in enumerate(self.dma_sems):
                if si in self.bg_sems:
                    continue
                if c > 0 and self.seen_dma[e].get(si, 0) < c:
                    self.seen_dma[e][si] = c
                    o.waits.append(("d", si, c))
            self.ops[e].append(o)

    def mm(self, out, pairs, extra_reads=()):
        oa = out.ap
        aps = [(l.ap, r.ap) for (l, r) in pairs]
        n = len(aps)

        def fn(e):
            ins = None
            for i, (l, r) in enumerate(aps):
                ins = e.matmul(oa, l, r, start=(i == 0), stop=(i == n - 1))
            return ins
        rd = [x for p in pairs for x in p] + list(extra_reads)
        return self.op("pe", fn, reads=rd, writes=[out])

    def tr(self, out, in_, ident):
        oa, ia, da = out.ap, in_.ap, ident.ap
        return self.op("pe", lambda e: e.transpose(oa, ia, da), reads=[in_, ident], writes=[out])

    def act(self, out, in_, func, bias=None, scale=None, accum=None, eng="act"):
        kw = {}
        rd = [in_]
        wr = [out]
        if bias is not None:
            if isinstance(bias, T):
                kw["bias"] = bias.ap
                rd.append(bias)
            else:
                kw["bias"] = bias
        if scale is not None:
            if isinstance(scale, T):
                kw["scale"] = scale.ap
                rd.append(scale)
            else:
                kw["scale"] = scale
        if accum is not None:
            kw["accum_out"] = accum.ap
            wr.append(accum)
        oa, ia = out.ap, in_.ap
        return self.op("act", lambda e: e.activation(out=oa, in_=ia, func=func, **kw), reads=rd, writes=wr)

    def tt(self, eng, out, in0, in1, op):
        oa, a, b = out.ap, in0.ap, in1.ap
        return self.op(eng, lambda e: e.tensor_tensor(out=oa, in0=a, in1=b, op=op), reads=[in0, in1], writes=[out])

    def ts(self, eng, out, in0, s1, s2, op0, op1=None):
        rd = [in0]
        a1 = s1
        a2 = s2
        if isinstance(s1, T):
            rd.append(s1)
            a1 = s1.ap
        if isinstance(s2, T):
            rd.append(s2)
            a2 = s2.ap
        oa, ia = out.ap, in0.ap
        if op1 is None:
            return self.op(eng, lambda e: e.tensor_scalar(out=oa, in0=ia, scalar1=a1, scalar2=None, op0=op0),
                           reads=rd, writes=[out])
        return self.op(eng, lambda e: e.tensor_scalar(out=oa, in0=ia, scalar1=a1, scalar2=a2, op0=op0, op1=op1),
                       reads=rd, writes=[out])

    def stt(self, eng, out, in0, scalar, in1, op0, op1):
        rd = [in0, in1]
        sa = scalar
        if isinstance(scalar, T):
            rd.append(scalar)
            sa = scalar.ap
        oa, a, b = out.ap, in0.ap, in1.ap
        return self.op(eng, lambda e: e.scalar_tensor_tensor(out=oa, in0=a, scalar=sa, in1=b, op0=op0, op1=op1),
                       reads=rd, writes=[out])

    def copy(self, eng, out, in_):
        oa, ia = out.ap, in_.ap
        if eng == "act":
            return self.op(eng, lambda e: e.activation(out=oa, in_=ia, func=AF.Copy), reads=[in_], writes=[out])
        return self.op(eng, lambda e: e.tensor_copy(out=oa, in_=ia), reads=[in_], writes=[out])

    def memset(self, eng, out, val):
        oa = out.ap
        return self.op(eng, lambda e: e.memset(oa, val), writes=[out])

    def emit(self):
        nc = self.nc
        csem = {}
        for e in ("pe", "act", "dve", "pool"):
            csem[e] = nc.alloc_semaphore(name=f"cs_{e}")
            c = 0
            for o in self.ops[e]:
                if o.signals:
                    c += 1
                    o.sigval = c
        dsem = self.dma_sems
        final_waits = [(h, c) for (h, c) in dsem if c > 0]

        def run(ename, eobj):
            for o in self.ops[ename]:
                for w in o.waits:
                    if w[0] == "d":
                        eobj.wait_ge(dsem[w[1]][0], w[2])
                    else:
                        d = w[1]
                        eobj.wait_ge(csem[d.eng], d.sigval)
                ins = o.fn(eobj)
                if o.is_dma:
                    ins.then_inc(dsem[o.sem][0], 16)
                elif o.signals:
                    ins.then_inc(csem[ename], 1)
            if ename == "sp":
                for (h, c) in final_waits:
                    eobj.wait_ge(h, c)

        with nc.Block() as block:
            @block.tensor
            def _(e):
                run("pe", e)

            @block.scalar
            def _(e):
                run("act", e)

            @block.vector
            def _(e):
                run("dve", e)

            @block.gpsimd
            def _(e):
                run("pool", e)

            @block.sync
            def _(e):
                run("sp", e)


DIL = (1, 4, 16)
REL_BUCKETS = 32
REL_MAX_DIST = 2048
ATT_SCALE = 128 ** -0.5
MASKNEG = -30000.0

WIN_BLOCKS = []
for _i in range(3):
    WIN_BLOCKS.append(("aq", 0 + 512 * _i, 512))
for _i in range(3):
    WIN_BLOCKS.append(("ak", 1536 + 512 * _i, 512))
for _i in range(3):
    WIN_BLOCKS.append(("av", 3072 + 512 * _i, 512))
WIN_BLOCKS.append(("gq", 4608, 384))
WIN_BLOCKS.append(("gk", 4992, 384))
WIN_BLOCKS.append(("gv", 5376, 384))
WIN_BLOCKS.append(("gv", 5760, 384))
WIN_BLOCKS.append(("glow", 6144, 16))
WIN_BLOCKS.append(("cu", 6160, 384))
WIN_BLOCKS.append(("cu", 6544, 384))
WIN_BLOCKS.append(("cv", 6928, 384))
WIN_BLOCKS.append(("cv", 7312, 384))
for _i in range(12):
    WIN_BLOCKS.append(("gate", 7696 + 512 * _i, 512))
N_WIN = len(WIN_BLOCKS)
BLK_BR = N_WIN
BLK_WO = BLK_BR + 8
BLK_F1 = BLK_WO + 4
BLK_F2 = BLK_F1 + 22
NBLK = BLK_F2 + 16
BLKE = 8192


def _t5_bucket(dist):
    max_exact = REL_BUCKETS // 2
    d = np.maximum(dist, 1)
    large = max_exact + (np.log(d / max_exact) / np.log(REL_MAX_DIST / max_exact)
                         * (REL_BUCKETS - max_exact)).astype(np.int64)
    large = np.minimum(large, REL_BUCKETS - 1)
    return np.where(dist < max_exact, dist, large).astype(np.int32)


def _bias_index_tables():
    j = np.arange(128)[:, None]
    i = np.arange(128)[None, :]
    idx = np.zeros((12, 2, 128, 128), np.int64)
    mask = np.zeros((2, 128, 128), np.float32)
    dprev = i + 128 - j
    dcur = i - j
    mask[1] = np.where(dprev <= 128, 0.0, MASKNEG)
    mask[0] = np.where(dcur >= 0, 0.0, MASKNEG)
    for hd in range(12):
        dil = DIL[hd // 4]
        idx[hd, 1] = _t5_bucket(np.clip(dprev, 0, None) * dil)
        idx[hd, 0] = _t5_bucket(np.clip(dcur, 0, None) * dil)
    return idx, mask


class Dram:
    def __init__(self, nc, name, shape, dt, kind="Internal", ntiles=1):
        self.ap = nc.dram_tensor(name, shape, dt, kind=kind).ap()
        self.bufs = [Buf(f"{name}_{i}", "dram") for i in range(ntiles)]

    def t(self, key, tile=0):
        return T(self.ap[key], self.bufs[tile])

    def all(self):
        return list(self.bufs)


def build(S=4096, depth=4, dbg=(), stop=None):
    nc = bass.Bass("TRN2", target_bir_lowering=False)
    P = Prog(nc)
    NTT = S // 512
    NT = S // 128

    def ext_in(name, shape, dt=F32):
        return Dram(nc, name, shape, dt, kind="ExternalInput")

    def scr(name, shape, dt=BF16, ntiles=NTT):
        kind = "ExternalOutput" if name in dbg else "Internal"
        return Dram(nc, name, shape, dt, kind=kind, ntiles=ntiles)

    x_in = ext_in("x", [S, D])
    abias_in = ext_in("abias", [128, 12, 2, 128])
    amask_in = ext_in("amask", [128, 2, 128])
    cst_in = ext_in("cst", [128, 4, 128])
    g1_in = ext_in("g1", [depth, 128, 16])
    g2_in = ext_in("g2", [depth, 128, 16])
    qkg_in = ext_in("qkg", [depth, 128, 2])
    gup_in = ext_in("gup", [depth, 16, 384])
    gb_in = ext_in("gb", [depth, 128, 384])
    gog_in = ext_in("gog", [depth, 96, 2])
    lng_in = ext_in("lng", [depth, 128, 768])
    lnb_in = ext_in("lnb", [depth, 128, 768])
    sw_in = ext_in("sw", [depth, 4, 128, 128])
    sb_in = ext_in("sbias", [depth, 1, 512])
    w_in = ext_in("w_in", [depth, D, D_IN])
    w_br = ext_in("w_branch", [depth, D, D])
    w_o = ext_in("w_out", [depth, D, D])
    w_f1 = ext_in("w_ffn_in", [depth, D, 2 * D_FFN])
    w_f2 = ext_in("w_ffn_out", [depth, D_FFN, D])
    y_out = Dram(nc, "y", [S, D], F32, kind="ExternalOutput", ntiles=NTT)

    xres = scr("xres", [128, 16, S], F32)
    wsc = [Dram(nc, f"wsc{p}", [NBLK, 128, BLKE], BF16, ntiles=NBLK) for p in range(2)]
    wsem = [[Buf(f"wsem{p}_{b}", "dram") for b in range((N_WIN + 4) if p == 0 else 8)] for p in range(2)]
    for p_ in range(2):
        for b_ in wsem[p_]:
            b_.sem = P.new_dma_sem()
            P.bg_sems.add(b_.sem)
    aq = scr("aq", [128, 12, S])
    ak = scr("ak", [128, 12, S])
    av = scr("av", [128, 12, S])
    gq = scr("gq", [96, 4, S])
    gk = scr("gk", [96, 4, S])
    gv = scr("gv", [96, 8, S])
    glow = scr("glow", [16, S])
    su = scr("su", [96, 8, S])
    vtok = scr("vtok", [NT, 128, 768])
    sg = scr("sg", [128, 48, S])
    oa = scr("oa", [128, 4, S])
    ob = scr("ob", [96, 8, S])
    oc = scr("oc", [96, 8, S])

    def sb(name, shape, dt):
        return T(nc.alloc_sbuf_tensor(name, shape, dt).ap(), Buf(name, "sb"))

    CST = sb("CST", [128, 4, 128], F32)
    IDB = sb("IDB", [128, 128], BF16)
    ONES = sb("ONES", [128, 128], BF16)
    TRIU = sb("TRIU", [128, 128], BF16)
    TRIR = sb("TRIR", [128, 128], BF16)
    ABT = sb("ABT", [128, 12, 2, 128], BF16)
    G1 = sb("G1", [128, 16], F32)
    G2 = sb("G2", [128, 16], F32)
    QKG = sb("QKG", [128, 2], F32)
    WUP = sb("WUP", [16, 384], BF16)
    GB = sb("GB", [128, 384], F32)
    GOG = sb("GOG", [96, 2], F32)
    LNG = sb("LNG", [128, 768], F32)
    LNB = sb("LNB", [128, 768], F32)
    WST = sb("WST", [128, 4, 128], BF16)
    BSH = sb("BSH", [1, 512], BF16)
    BSL = sb("BSL", [1, 512], BF16)

    ARENA_BYTES = nc.sbuf_bytes_remaining - 2048
    arena = nc.alloc_sbuf_tensor("arena", [128, ARENA_BYTES], mybir.dt.uint8).ap()

    class Arena:
        def __init__(self):
            self.off = 0
            P.reset_pool()

        def alloc(self, name, shape, dt):
            esz = 2 if dt == BF16 else 4
            n = int(np.prod(shape[1:])) * esz
            n = (n + 63) // 64 * 64
            assert self.off + n <= ARENA_BYTES, (name, self.off, n, ARENA_BYTES)
            ap = arena[:, self.off:self.off + n].bitcast(dt)
            ap = ap[:, 0:int(np.prod(shape[1:]))]
            if len(shape) == 3:
                ap = ap.rearrange("p (a b) -> p a b", a=shape[1])
            elif len(shape) == 4:
                ap = ap.rearrange("p (a b c) -> p a b c", a=shape[1], b=shape[2])
            ap = ap[0:shape[0]]
            self.off += n
            return T(ap, Buf(name, "sb", True))

    PSB = [T(nc.alloc_psum_tensor(f"ps{i}", [128, 512], F32).ap(), Buf(f"ps{i}", "ps")) for i in range(8)]

    pw = ALU.pow
    MUL = ALU.mult
    ADD = ALU.add

    def rsqrt_inplace(eng, t, in_, mulc):
        P.act(t, in_, AF.Ln, bias=EPS, scale=mulc)
        P.act(t, t, AF.Exp, scale=-0.5)

    P.dma("sp", CST, cst_in.t(slice(None)))
    P.copy("dve", IDB, CST[:, 0, :])
    P.copy("dve", TRIU, CST[:, 1, :])
    P.copy("dve", TRIR, CST[:, 2, :])
    P.memset("dve", ONES, 1.0)

    def wsem_idx(par, blk):
        if par == 0:
            return blk if blk < N_WIN else N_WIN + (blk % 4)
        return (blk % 4) if blk < N_WIN else 4 + (blk % 4)

    def convert(l):
        par = l % 2
        W = wsc[par]

        def cv(blk, out_ap, in_ap):
            P.dma("pool", T(out_ap, W.bufs[blk]), T(in_ap, Buf("wext", "dram")), sem_t=wsem[par][wsem_idx(par, blk)])
        for bi, (kind, c0, w) in enumerate(WIN_BLOCKS):
            cv(bi, W.ap[bi, :, 0:16 * w].rearrange("p (kc c) -> p kc c", kc=16),
               w_in.ap[l, :, c0:c0 + w].rearrange("(kc p) c -> p kc c", p=128))
        for b in range(8):
            c0 = b * 256
            blk = BLK_BR + b
            cv(blk, W.ap[blk, :, 0:1024].rearrange("p (kc c) -> p kc c", kc=4),
               w_br.ap[l, 0:512, c0:c0 + 256].rearrange("(kc p) c -> p kc c", p=128))
            cv(blk, W.ap[blk, 0:96, 1024:3072].rearrange("p (kc c) -> p kc c", kc=8),
               w_br.ap[l, 512:1280, c0:c0 + 256].rearrange("(kc p) c -> p kc c", p=96))
            cv(blk, W.ap[blk, 0:96, 3072:5120].rearrange("p (kc c) -> p kc c", kc=8),
               w_br.ap[l, 1280:2048, c0:c0 + 256].rearrange("(kc p) c -> p kc c", p=96))
        for b in range(4):
            blk = BLK_WO + b
            cv(blk, W.ap[blk, :, :].rearrange("p (kc c) -> p kc c", kc=16),
               w_o.ap[l, :, b * 512:(b + 1) * 512].rearrange("(kc p) c -> p kc c", p=128))
        for b in range(22):
            blk = BLK_F1 + b
            o4 = W.ap[blk, :, :].rearrange("p (kc t c) -> p kc t c", kc=16, t=2)
            cv(blk, o4[:, :, 0, :], w_f1.ap[l, :, b * 256:(b + 1) * 256].rearrange("(kc p) c -> p kc c", p=128))
            cv(blk, o4[:, :, 1, :],
               w_f1.ap[l, :, D_FFN + b * 256:D_FFN + (b + 1) * 256].rearrange("(kc p) c -> p kc c", p=128))
        for hf in range(2):
            for b in range(8):
                blk = BLK_F2 + hf * 8 + b
                cv(blk, W.ap[blk, :, 0:22 * 256].rearrange("p (kc c) -> p kc c", kc=22),
                   w_f2.ap[l, hf * 2816:(hf + 1) * 2816, b * 256:(b + 1) * 256].rearrange("(kc p) c -> p kc c", p=128))

    class WStream:
        def __init__(self, slots, seq, look=2):
            self.slots = slots
            self.seq = seq
            self.nxt = 0
            self.look = look

        def get(self, i):
            lim = min(i + self.look, len(self.seq) - 1)
            while self.nxt <= lim:
                par, blk, ne = self.seq[self.nxt]
                slot = self.slots[self.nxt % len(self.slots)]
                P.dma("sp", slot[:, 0:ne], wsc[par].t((blk, slice(None), slice(0, ne)), blk))
                self.nxt += 1
            return self.slots[i % len(self.slots)]

    def setup_x():
        A = Arena()
        XT = [A.alloc(f"XT{i}", [128, D], F32) for i in range(2)]
        XO = [A.alloc(f"XO{i}", [128, 16, 128], F32) for i in range(2)]
        for t in range(NT):
            xt = XT[t % 2]
            xo = XO[t % 2]
            P.dma("sp", xt, x_in.t((slice(t * 128, (t + 1) * 128), slice(None))))
            for q in range(4):
                ps = PSB[(t * 4 + q) % 8]
                for j in range(4):
                    kc = q * 4 + j
                    P.tr(ps[:, j * 128:(j + 1) * 128], xt[:, kc * 128:(kc + 1) * 128], CST[:, 0, :])
                P.copy("act" if q % 2 == 0 else "dve", xo[:, q * 4:(q + 1) * 4, :],
                       ps.re("p (a b) -> p a b", a=4))
            P.dma("sp", xres.t((slice(None), slice(None), slice(t * 128, (t + 1) * 128)), t // 4), xo)

    def load_params(l):
        A = Arena()
        P.dma("sp", G1, g1_in.t(l))
        P.dma("sp", G2, g2_in.t(l))
        P.dma("sp", QKG, qkg_in.t(l))
        WUP32 = A.alloc("WUP32", [16, 384], F32)
        P.dma("sp", WUP32, gup_in.t(l))
        P.copy("dve", WUP, WUP32)
        P.dma("sp", GB, gb_in.t(l))
        P.dma("sp", GOG, gog_in.t(l))
        P.dma("sp", LNG, lng_in.t(l))
        P.dma("sp", LNB, lnb_in.t(l))
        B32 = A.alloc("B32", [1, 512], F32)
        BH32 = A.alloc("BH32", [1, 512], F32)
        P.dma("sp", B32, sb_in.t(l))
        P.copy("dve", BSH, B32)
        P.copy("dve", BH32, BSH)
        P.tt("dve", BSL, B32, BH32, ALU.subtract)
        W32 = A.alloc("W32", [128, 4, 128], F32)
        WB = A.alloc("WB", [128, 4, 128], BF16)
        P.dma("sp", W32, T(sw_in.ap[l].rearrange("g t s -> t g s"), sw_in.bufs[0]))
        for g in range(4):
            P.tt("dve", WB[:, g, :], W32[:, g, :], CST[:, 3, :], MUL)
        psb = PSB[0].bc(BF16)
        for g in range(4):
            P.tr(psb[:, g * 128:(g + 1) * 128], WB[:, g, :], IDB)
        P.copy("dve", WST, psb[:, 0:512].re("p (g t) -> p g t", g=4))
        if l == 0:
            AB32 = A.alloc("AB32", [128, 12, 2, 128], F32)
            AM32 = A.alloc("AM32", [128, 2, 128], F32)
            P.dma("sp", AB32, abias_in.t(slice(None)))
            P.dma("sp", AM32, amask_in.t(slice(None)))
            for hd in range(12):
                P.stt("dve", ABT[:, hd], AB32[:, hd], float(128 ** 0.5), AM32, MUL, ADD)

    def norm_sq(XS, HT, kcs=range(16)):
        for kc in kcs:
            P.act(HT[:, kc, :], XS[:, kc, :], AF.Square)

    def norm_fin(XS, HT, G, RSTD, psx, SQ=None, use_pool=False):
        SQ = HT if SQ is None else SQ
        P.mm(psx, [(ONES, SQ[:, kc, :]) for kc in range(16)])
        rsqrt_inplace("dve", RSTD, psx, 1.0 / D)
        for kc in range(16):
            P.stt("pool" if (use_pool and kc % 2 == 1) else "dve", HT[:, kc, :], XS[:, kc, :], G[:, kc:kc + 1], RSTD, MUL, MUL)

    def rmsnorm_tile(XS, HT, G, RSTD, psx):
        norm_sq(XS, HT)
        norm_fin(XS, HT, G, RSTD, psx)

    def phase1(l):
        par = l % 2
        A = Arena()
        XSs = [A.alloc(f"XS{i}", [128, 16, 512], F32) for i in range(2)]
        HTs = [A.alloc(f"HT{i}", [128, 16, 512], BF16) for i in range(2)]
        WS = [A.alloc(f"WS{i}", [128, BLKE], BF16) for i in range(3)]
        RSTD = A.alloc("RSTD", [128, 512], F32)
        SQa = [A.alloc(f"SQa{i}", [128, 512], BF16) for i in range(2)]
        R = [A.alloc(f"R{i}", [128, 512], F32) for i in range(2)]
        OST = [A.alloc(f"OST{i}", [128, 512], BF16) for i in range(6)]
        CVG = A.alloc("CVG", [128, 4, 768], F32)
        TMP = [A.alloc(f"TMP{i}", [128, 768], F32) for i in range(2)]
        VTO = [A.alloc(f"VTO{i}", [128, 768], BF16) for i in range(2)]
        STT = A.alloc("STT", [128, 2, 6], F32)
        MV = A.alloc("MV", [128, 2], F32)
        RS = A.alloc("RS", [128, 1], F32)
        seq = []
        for tt in range(NTT):
            for bi, (kind, c0, w) in enumerate(WIN_BLOCKS):
                seq.append((par, bi, 16 * w))
        ws = WStream(WS, seq)
        ctr = {"ps": 0, "ost": 0, "aux": 0, "i": 0}

        def next_ps():
            ctr["ps"] += 1
            return PSB[ctr["ps"] % 4]

        def next_aux():
            ctr["aux"] += 1
            return PSB[4 + ctr["aux"] % 2]

        def next_ost():
            ctr["ost"] += 1
            return OST[ctr["ost"] % 6]

        pending = []

        def flush_pending():
            while pending:
                pending.pop(0)()

        P.dma("sp", XSs[0], xres.t((slice(None), slice(None), slice(0, 512)), 0))
        rmsnorm_tile(XSs[0], HTs[0], G1, RSTD, PSB[6])
        for tt in range(NTT):
            tok = slice(tt * 512, (tt + 1) * 512)
            XS, HT = XSs[tt % 2], HTs[tt % 2]
            XSn, HTn = XSs[(tt + 1) % 2], HTs[(tt + 1) % 2]
            for bi, (kind, c0, w) in enumerate(WIN_BLOCKS):
                if tt + 1 < NTT:
                    if bi == 0:
                        P.dma("sp", XSn, xres.t((slice(None), slice(None), slice((tt + 1) * 512, (tt + 2) * 512)), tt + 1))
                    elif bi == 6:
                        norm_sq(XSn, HTn)
                    elif bi == 14:
                        norm_fin(XSn, HTn, G1, RSTD, PSB[6])
                slot = ws.get(ctr["i"])
                ctr["i"] += 1
                sw = slot[:, 0:16 * w].re("p (kc c) -> p kc c", kc=16)
                if kind in ("aq", "ak", "av", "gate"):
                    for m in range(4):
                        ps = next_ps()
                        P.mm(ps, [(sw[:, kc, m * 128:(m + 1) * 128], HT[:, kc, :]) for kc in range(16)])
                        o = next_ost()
                        flush_pending()
                        if kind in ("aq", "ak"):
                            base = 0 if kind == "aq" else 1536
                            hd = (c0 - base) // 128 + m
                            qa = SQa[hd % 2]
                            r = R[hd % 2]
                            P.act(qa, ps, AF.Square)

                            def tail(kind=kind, hd=hd, qa=qa, r=r, ps=ps, o=o, tok=tok, tt=tt):
                                px = next_aux()
                                P.mm(px, [(ONES, qa)])
                                rsqrt_inplace("dve", r, px, 1.0 / 128)
                                P.stt("dve", o, ps, QKG[:, (0 if kind == "aq" else 1):(1 if kind == "aq" else 2)], r, MUL, MUL)
                                dst = (aq if kind == "aq" else ak)
                                P.dma("sp", dst.t((slice(None), hd, tok), tt), o)
                            pending.append(tail)
                        elif kind == "av":
                            hd = (c0 - 3072) // 128 + m
                            P.copy("act", o, ps)
                            P.dma("sp", av.t((slice(None), hd, tok), tt), o)
                        else:
                            gi = (c0 - 7696) // 128 + m
                            P.act(o, ps, AF.Sigmoid)
                            P.dma("sp", sg.t((slice(None), gi, tok), tt), o)
                elif kind in ("gq", "gk", "gv", "cu"):
                    for m in range(4):
                        ps = next_ps()
                        P.mm(ps[0:96], [(sw[:, kc, m * 96:(m + 1) * 96], HT[:, kc, :]) for kc in range(16)])
                        flush_pending()
                        o = next_ost()
                        if kind == "gq":
                            P.act(o[0:96], ps[0:96], AF.Copy, scale=float(96 ** -0.5))
                            P.dma("sp", gq.t((slice(None), m, tok), tt), o[0:96])
                        elif kind == "gk":
                            P.copy("act", o[0:96], ps[0:96])
                            P.dma("sp", gk.t((slice(None), m, tok), tt), o[0:96])
                        elif kind == "gv":
                            j = (c0 - 5376) // 96 + m
                            P.copy("act", o[0:96], ps[0:96])
                            P.dma("sp", gv.t((slice(None), j, tok), tt), o[0:96])
                        else:
                            j = (c0 - 6160) // 96 + m
                            P.act(o[0:96], ps[0:96], AF.Gelu)
                            P.dma("sp", su.t((slice(None), j, tok), tt), o[0:96])
                elif kind == "glow":
                    ps = next_ps()
                    P.mm(ps[0:16], [(sw[:, kc, 0:16], HT[:, kc, :]) for kc in range(16)])
                    o = next_ost()
                    P.copy("act", o[0:16], ps[0:16])
                    P.dma("sp", glow.t((slice(None), tok), tt), o[0:16])
                elif kind == "cv":
                    half = (c0 - 6928) // 384
                    for s in range(4):
                        ps = next_ps()
                        P.mm(ps[:, 0:384], [(HT[:, kc, s * 128:(s + 1) * 128], sw[:, kc, 0:384]) for kc in range(16)])
                        P.act(CVG[:, s, half * 384:(half + 1) * 384], ps[:, 0:384], AF.Gelu)
                    if half == 1:
                        for s in range(4):
                            tm = TMP[s % 2]
                            vo = VTO[s % 2]
                            c_ = CVG[:, s, :]
                            a0, a1 = STT[:, 0, :].ap, STT[:, 1, :].ap
                            i0, i1 = CVG[:, s, 0:384].ap, CVG[:, s, 384:768].ap
                            P.op("dve", lambda e, a0=a0, i0=i0: e.bn_stats(out=a0, in_=i0), reads=[CVG], writes=[STT])
                            P.op("dve", lambda e, a1=a1, i1=i1: e.bn_stats(out=a1, in_=i1), reads=[CVG], writes=[STT])
                            mva, sta = MV.ap, STT.re("p a b -> p (a b)").ap
                            P.op("dve", lambda e, mva=mva, sta=sta: e.bn_aggr(out=mva, in_=sta), reads=[STT], writes=[MV])
                            P.act(RS, MV[:, 1:2], AF.Ln, bias=EPS)
                            P.act(RS, RS, AF.Exp, scale=-0.5)
                            P.ts("dve", tm, c_, MV[:, 0:1], RS, ALU.subtract, MUL)
                            P.tt("dve", tm, tm, LNG, MUL)
                            P.tt("dve", vo, tm, LNB, ADD)
                            P.dma("sp", vtok.t(tt * 4 + s, tt), vo)

    def phase2_attn(l, A):
        QKV = [[A.alloc(f"QKV{i}{j}", [128, S], BF16) for j in range(3)] for i in range(2)]
        ACC = A.alloc("ACC", [128, 2, S], F32)
        ACCb = [Buf(f"ACC{c}", "sb", True) for c in range(NTT)]
        VT = [A.alloc(f"VT{i}", [128, 32, 128], BF16) for i in range(2)]
        PT = [A.alloc(f"PT{i}", [128, 2, 128], BF16) for i in range(4)]
        RC = [A.alloc(f"RC{i}", [128, 512], F32) for i in range(2)]
        OAS = [A.alloc(f"OAS{i}", [128, 512], BF16) for i in range(2)]
        cnt = 0
        pc = 0
        combos = [(hs_, g_) for hs_ in range(4) for g_ in range(3)]

        def load_qkv(ci):
            hs_, g_ = combos[ci]
            hd_ = g_ * 4 + hs_
            Q_, K_, V_ = QKV[ci % 2]
            P.dma("sp", Q_, T(aq.ap[:, hd_, :], aq.bufs[0]), xr=aq.all())
            P.dma("sp", K_, T(ak.ap[:, hd_, :], ak.bufs[0]), xr=ak.all())
            P.dma("sp", V_, T(av.ap[:, hd_, :], av.bufs[0]), xr=av.all())
        load_qkv(0)
        for hs in range(4):
            for c in range(NTT):
                for u_ in range(2):
                    ma = ACC[:, u_, c * 512:(c + 1) * 512].ap
                    P.op("pool", lambda e, ma=ma: e.memset(ma, 0.0), writes=[ACCb[c]])
            for g in range(3):
                hd = g * 4 + hs
                d = DIL[g]
                nb = (S // d) // 128
                Q, K, V = QKV[cnt % 2]
                vt = VT[cnt % 2]
                cnt += 1
                if cnt < len(combos):
                    load_qkv(cnt)

                def blk(r, n, d=d):
                    st = r + d * 128 * n
                    return slice(st, st + d * 127 + 1, d)
                if g == 2:
                    bl = [(r, n) for n in range(nb) for r in range(d)]
                else:
                    bl = [(r, n) for r in range(d) for n in range(nb)]
                bidx = {rn: i for i, rn in enumerate(bl)}
                for q4 in range(8):
                    psb = PSB[2 + q4 % 2].bc(BF16)
                    for j in range(4):
                        r, n = bl[q4 * 4 + j]
                        P.tr(psb[:, j * 128:(j + 1) * 128], V[:, blk(r, n)], IDB)
                    P.copy("act" if q4 % 2 == 0 else "dve", vt[:, q4 * 4:(q4 + 1) * 4, :],
                           psb[:, 0:512].re("p (a b) -> p a b", a=4))
                    yield

                def qsl_of(bi):
                    r, n = bl[bi]
                    w = 256 if n + 1 < nb else 128
                    st = r + d * 128 * n
                    return slice(st, st + d * (w - 1) + 1, d), w

                def stage_a(bi):
                    r, n = bl[bi]
                    qs, w = qsl_of(bi)
                    pss = PSB[bi % 2]
                    pt = PT[bi % 4].re("p a b -> p (a b)")
                    P.mm(pss[:, 0:w], [(K[:, blk(r, n)], Q[:, qs]),
                                       (IDB, ABT[:, hd].re("p a b -> p (a b)")[:, 0:w])])
                    P.act(pt[:, 0:w], pss[:, 0:w], AF.Exp, scale=ATT_SCALE)

                def stage_b(bi):
                    qs, w = qsl_of(bi)
                    pso = PSB[2 + bi % 2].re("p (a b) -> p a b", a=2)
                    pt = PT[bi % 4].re("p a b -> p (a b)")
                    P.mm(pso[:, 0, 0:w], [(vt[:, bi, :], pt[:, 0:w])])
                    P.mm(pso[:, 1, 0:w], [(ONES, pt[:, 0:w])])
                    r, n = bl[bi]
                    st = r + d * 128 * n
                    en = st + d * (w - 1)
                    cb = [ACCb[c] for c in range(st // 512, en // 512 + 1)]
                    aa, pa_ = ACC[:, :, qs].ap, pso[:, :, 0:w].ap
                    P.op("dve", lambda e, aa=aa, pa_=pa_: e.tensor_tensor(out=aa, in0=aa, in1=pa_, op=ADD),
                         reads=[pso] + cb, writes=cb)
                stage_a(0)
                stage_a(1)
                for bi in range(32):
                    if bi + 2 < 32:
                        stage_a(bi + 2)
                    stage_b(bi)
                    yield
            for c in range(NTT):
                tok = slice(c * 512, (c + 1) * 512)
                rc = RC[c % 2]
                o = OAS[c % 2]
                rca, ia = rc.ap, ACC[:, 1, tok].ap
                P.op("dve", lambda e, rca=rca, ia=ia: e.reciprocal(out=rca, in_=ia), reads=[ACCb[c]], writes=[rc])
                oa_, ua = o.ap, ACC[:, 0, tok].ap
                P.op("dve", lambda e, oa_=oa_, ua=ua, rca=rca: e.tensor_tensor(out=oa_, in0=ua, in1=rca, op=MUL),
                     reads=[ACCb[c], rc], writes=[o])
                P.dma("pool", oa.t((slice(None), hs, tok), c), o)
            yield

    def phase2_gla(l, A):
        GQ = [A.alloc(f"GQ{i}", [96, 4, 512], BF16) for i in range(1)] * 2
        GK = [A.alloc(f"GK{i}", [96, 4, 512], BF16) for i in range(1)] * 2
        GV = [A.alloc(f"GV{i}", [96, 8, 512], BF16) for i in range(1)] * 2
        GL = [A.alloc(f"GL{i}", [16, 512], BF16) for i in range(2)]
        PRE = A.alloc("PRE", [128, 384], F32)
        EE = A.alloc("EE", [128, 384], F32)
        LB = A.alloc("LB", [128, 384], BF16)
        EBP = [A.alloc(f"EBP{i}", [96, 4, 128], F32) for i in range(2)]
        EBN = A.alloc("EBN", [96, 4, 128], F32)
        ERB = A.alloc("ERB", [128, 384], F32)
        QT = [A.alloc(f"QT{i}", [96, 4, 128], BF16) for i in range(2)]
        KT = [A.alloc(f"KT{i}", [96, 4, 128], BF16) for i in range(2)]
        KH = [A.alloc(f"KH{i}", [128, 384], BF16) for i in range(2)]
        VTK = [A.alloc(f"VTK{i}", [128, 768], BF16) for i in range(2)]
        STM = [A.alloc(f"STM{i}", [128, 128], BF16) for i in range(4)]
        SF = [A.alloc(f"SF{h}", [96, 192], F32) for h in range(4)]
        SBF = [[A.alloc(f"SBF{h}{i}", [96, 192], BF16) for i in range(2)] for h in range(4)]
        SQG = A.alloc("SQG", [96, 8, 128], BF16)
        RN = A.alloc("RN", [96, 512], F32)
        OBS = [A.alloc(f"OBS{i}", [96, 8, 512], BF16) for i in range(1)] * 2
        B4, B5, B6, B7 = PSB[4], PSB[5], PSB[6], PSB[7]
        for c in range(NT):
            tt, s4 = c // 4, c % 4
            tok = slice(tt * 512, (tt + 1) * 512)
            sl = slice(s4 * 128, (s4 + 1) * 128)
            q_, k_, v_, gl_ = GQ[tt % 2], GK[tt % 2], GV[tt % 2], GL[tt % 2]
            obs = OBS[tt % 2]
            if s4 == 0:
                P.dma("sp", q_, gq.t((slice(None), slice(None), tok), tt))
                P.dma("sp", k_, gk.t((slice(None), slice(None), tok), tt))
                P.dma("sp", v_, gv.t((slice(None), slice(None), tok), tt))
                P.dma("sp", gl_, glow.t((slice(None), tok), tt))
            ebp, qt, kt, kh, vtk = EBP[c % 2], QT[c % 2], KT[c % 2], KH[c % 2], VTK[c % 2]
            P.mm(B4[:, 0:384], [(gl_[:, sl], WUP)])
            P.tt("dve", PRE, B4[:, 0:384], GB, ADD)
            P.act(EE, PRE, AF.Exp, scale=-1.0)
            P.act(LB, EE, AF.Ln, bias=1.0)
            psbt = B5.re("p (a b) -> p a b", a=4)
            for h in range(4):
                P.mm(psbt[0:96, h, :], [(LB[:, h * 96:(h + 1) * 96], TRIU)])
            P.mm(B4[:, 0:384], [(TRIR, LB)])
            P.act(ebp, psbt[0:96], AF.Exp, scale=-1.0 / 16)
            P.act(EBN, psbt[0:96], AF.Exp, scale=1.0 / 16)
            P.act(ERB, B4[:, 0:384], AF.Exp, scale=-1.0 / 16)
            P.tt("dve", qt, q_[:, :, sl], ebp, MUL)
            P.tt("dve", kt, k_[:, :, sl], EBN, MUL)
            pk = B5.bc(BF16)
            for h in range(4):
                P.tr(pk[:, h * 96:(h + 1) * 96], k_[:, h, sl], IDB[0:96, 0:96])
            pv = B6.bc(BF16)
            for j in range(8):
                P.tr(pv[:, j * 96:(j + 1) * 96], v_[:, j, sl], IDB[0:96, 0:96])
            P.tt("dve", kh, pk[:, 0:384], ERB, MUL)
            P.copy("act", vtk, pv[:, 0:768])
            yield
            pso = [B6.re("p (a b) -> p a b", a=4), B7.re("p (a b) -> p a b", a=4)]
            for h in range(4):
                stm = STM[h]
                P.mm(B4[:, 0:128], [(kt[:, h, :], qt[:, h, :])])
                P.tt("dve", stm, B4[:, 0:128], TRIU, MUL)
                sbf_old = SBF[h][(c + 1) % 2]
                sbf_new = SBF[h][c % 2]
                for j in range(2):
                    prs = [(vtk[:, h * 192 + j * 96:h * 192 + (j + 1) * 96], stm)]
                    if c > 0:
                        prs.append((sbf_old[:, j * 96:(j + 1) * 96], qt[:, h, :]))
                    P.mm(pso[h // 2][0:96, (h % 2) * 2 + j, :], prs)
                P.mm(B5[0:96, 192:384], [(kh[:, h * 96:(h + 1) * 96], vtk[:, h * 192:(h + 1) * 192])])
                if c == 0:
                    P.copy("dve", SF[h], B5[0:96, 192:384])
                else:
                    P.stt("dve", SF[h], SF[h], ebp[:, h, 127:128], B5[0:96, 192:384], MUL, ADD)
                if c < NT - 1:
                    P.copy("act", sbf_new, SF[h])
                if h == 1:
                    yield
            P.act(SQG[:, 0:4, :], pso[0][0:96], AF.Square)
            P.act(SQG[:, 4:8, :], pso[1][0:96], AF.Square)
            for h in range(4):
                P.mm(B4[0:96, h * 128:(h + 1) * 128],
                     [(ONES[0:96, 0:96], SQG[:, 2 * h, :]), (ONES[0:96, 0:96], SQG[:, 2 * h + 1, :])])
            rsqrt_inplace("dve", RN, B4[0:96, :], 1.0 / 192)
            for h in range(4):
                for j in range(2):
                    P.stt("dve", obs[:, 2 * h + j, sl], pso[h // 2][0:96, (h % 2) * 2 + j, :], GOG[:, j:j + 1],
                          RN[:, h * 128:(h + 1) * 128], MUL, MUL)
            if s4 == 3:
                P.dma("pool", ob.t((slice(None), slice(None), tok), tt), obs)
            yield

    def phase2_sgu(l, A):
        SU = [A.alloc(f"SU{i}", [96, 8, 512], BF16) for i in range(1)] * 2
        VK = [A.alloc(f"VK{i}", [128, 768], BF16) for i in range(3)]
        OCS = [A.alloc(f"OCS{i}", [96, 8, 512], BF16) for i in range(1)] * 2
        for c in range(NT):
            tt, s4 = c // 4, c % 4
            tok = slice(tt * 512, (tt + 1) * 512)
            sl = slice(s4 * 128, (s4 + 1) * 128)
            su_, ocs, vk = SU[tt % 2], OCS[tt % 2], VK[c % 3]
            if s4 == 0:
                P.dma("sp", su_, su.t((slice(None), slice(None), tok), tt))
            P.dma("sp", vk, vtok.t(c, tt))
            for hf in range(2):
                psf = PSB[hf].re("p (a b) -> p a b", a=4)
                for jj in range(4):
                    j = hf * 4 + jj
                    g = j // 2
                    P.mm(psf[0:96, jj, :], [(vk[:, j * 96:(j + 1) * 96], WST[:, g, :]),
                                            (ONES[0:1, 0:96], BSH[0:1, g * 128:(g + 1) * 128]),
                                            (ONES[0:1, 0:96], BSL[0:1, g * 128:(g + 1) * 128])])
                P.tt("dve", ocs[:, hf * 4:(hf + 1) * 4, sl], psf[0:96], su_[:, hf * 4:(hf + 1) * 4, sl], MUL)
            if s4 == 3:
                P.dma("pool", oc.t((slice(None), slice(None), tok), tt), ocs)
            yield

    def phase2(l):
        A = Arena()
        ga = phase2_attn(l, A)
        gg = phase2_gla(l, A)
        gs = phase2_sgu(l, A)
        live = {"a": ga, "g": gg, "s": gs}

        def step(k, n=1):
            for _ in range(n):
                if k in live:
                    try:
                        next(live[k])
                    except StopIteration:
                        del live[k]
        while live:
            step("a", 5)
            step("g", 1)
            step("s", 1 if "g" not in live or True else 0)
            step("a", 5)
            step("g", 1)
            step("a", 5)
            step("g", 1)

    def phase3(l, last):
        par = l % 2
        A = Arena()
        XS = A.alloc("XS", [128, 16, 512], F32)
        HT = A.alloc("HT", [128, 16, 512], BF16)
        WS = [A.alloc(f"WS{i}", [128, BLKE], BF16) for i in range(3)]
        RSTD = A.alloc("RSTD", [128, 512], F32)
        OA = A.alloc("OA", [128, 4, 512], BF16)
        OB = A.alloc("OB", [96, 8, 512], BF16)
        OC = A.alloc("OC", [96, 8, 512], BF16)
        SGT = [A.alloc(f"SGT{i}", [128, 3, 512], BF16) for i in range(3)]
        T1 = [A.alloc(f"T1{i}", [128, 512], F32) for i in range(2)]
        T2 = [A.alloc(f"T2{i}", [128, 512], F32) for i in range(2)]
        T3 = [A.alloc(f"T3{i}", [128, 512], F32) for i in range(2)]
        SIL = [A.alloc(f"SIL{i}", [128, 512], F32) for i in range(2)]
        ACTB = A.alloc("ACTB", [128, 22, 512], BF16)
        SQ3 = A.alloc("SQ3", [128, 16, 512], BF16)
        if last:
            yv = ACTB.re("p a b -> p (a b)").bc(F32)
            YO = [yv[:, 0:D], yv[:, D:2 * D]]
        else:
            YO = None
        seq = []
        for tt in range(NTT):
            seq += [(par, BLK_BR + b, 5120) for b in range(8)]
            seq += [(par, BLK_WO + b, 8192) for b in range(4)]
            for hf in range(2):
                seq += [(par, BLK_F1 + hf * 11 + b, 8192) for b in range(11)]
                seq += [(par, BLK_F2 + hf * 8 + b, 22 * 256) for b in range(8)]
        ws = WStream(WS, seq)
        wi = 0
        pc = 0
        sg4 = sg.ap.rearrange("p (b m) s -> p b m s", b=3)
        sgs = {"n": 0}

        def sgt_prefetch(upto):
            while sgs["n"] <= min(upto, NTT * 16 - 1):
                i = sgs["n"]
                t_, m_ = i // 16, i % 16
                P.dma("act", SGT[i % 3], T(sg4[:, :, m_, t_ * 512:(t_ + 1) * 512], sg.bufs[t_]))
                sgs["n"] += 1
        for tt in range(NTT):
            tok = slice(tt * 512, (tt + 1) * 512)
            if tt == 0:
                P.dma("act", OA, oa.t((slice(None), slice(None), tok), tt))
                P.dma("act", OB, ob.t((slice(None), slice(None), tok), tt))
                P.dma("act", OC, oc.t((slice(None), slice(None), tok), tt))
            for b in range(8):
                slot = ws.get(wi)
                wi += 1
                wa = slot[:, 0:1024].re("p (kc c) -> p kc c", kc=4)
                wb = slot[0:96, 1024:3072].re("p (kc c) -> p kc c", kc=8)
                wc = slot[0:96, 3072:5120].re("p (kc c) -> p kc c", kc=8)
                for mm_ in range(2):
                    m = b * 2 + mm_
                    cs = slice(mm_ * 128, (mm_ + 1) * 128)
                    sgt_prefetch(tt * 16 + m + 2)
                    sgt = SGT[(tt * 16 + m) % 3]
                    pa, pb, pcx = PSB[pc % 8], PSB[(pc + 1) % 8], PSB[(pc + 2) % 8]
                    pc += 3
                    P.mm(pa, [(wa[:, kc, cs], OA[:, kc, :]) for kc in range(4)])
                    P.mm(pb, [(wb[:, kc, cs], OB[:, kc, :]) for kc in range(8)])
                    P.mm(pcx, [(wc[:, kc, cs], OC[:, kc, :]) for kc in range(8)])
                    t1, t2, t3 = T1[m % 2], T2[m % 2], T3[m % 2]
                    P.tt("dve", t1, pa, sgt[:, 0, :], MUL)
                    P.tt("dve", t2, pb, sgt[:, 1, :], MUL)
                    P.tt("dve", t3, pcx, sgt[:, 2, :], MUL)
                    P.tt("pool", t1, t1, t2, ADD)
                    P.tt("pool", HT[:, m, :], t1, t3, ADD)
            P.dma("act", XS, xres.t((slice(None), slice(None), tok), tt))
            if tt + 1 < NTT:
                tokn = slice((tt + 1) * 512, (tt + 2) * 512)
                P.dma("act", OA, oa.t((slice(None), slice(None), tokn), tt + 1))
                P.dma("act", OB, ob.t((slice(None), slice(None), tokn), tt + 1))
                P.dma("act", OC, oc.t((slice(None), slice(None), tokn), tt + 1))
            for b in range(4):
                slot = ws.get(wi)
                wi += 1
                sw = slot.re("p (kc c) -> p kc c", kc=16)
                for mm_ in range(4):
                    m = b * 4 + mm_
                    ps = PSB[pc % 8]
                    pc += 1
                    P.mm(ps, [(sw[:, kc, mm_ * 128:(mm_ + 1) * 128], HT[:, kc, :]) for kc in range(16)])
                    P.tt("dve", XS[:, m, :], XS[:, m, :], ps, ADD)
                    P.act(SQ3[:, m, :], XS[:, m, :], AF.Square)
            norm_fin(XS, HT, G2, RSTD, PSB[pc % 8], SQ=SQ3, use_pool=False)
            pc += 1
            for hf in range(2):
                for b in range(11):
                    slot = ws.get(wi)
                    wi += 1
                    sw = slot.re("p (kc t c) -> p kc t c", kc=16, t=2)
                    for mm_ in range(2):
                        j = b * 2 + mm_
                        cs = slice(mm_ * 128, (mm_ + 1) * 128)
                        pg, pu = PSB[pc % 8], PSB[(pc + 1) % 8]
                        pc += 2
                        P.mm(pg, [(sw[:, kc, 0, cs], HT[:, kc, :]) for kc in range(16)])
                        P.mm(pu, [(sw[:, kc, 1, cs], HT[:, kc, :]) for kc in range(16)])
                        sil = SIL[j % 2]
                        P.act(sil, pg, AF.Silu)
                        P.tt("dve", ACTB[:, j, :], sil, pu, MUL)
                for b in range(8):
                    slot = ws.get(wi)
                    wi += 1
                    sw = slot[:, 0:22 * 256].re("p (kc c) -> p kc c", kc=22)
                    for mm_ in range(2):
                        m = b * 2 + mm_
                        ps = PSB[pc % 8]
                        pc += 1
                        P.mm(ps, [(sw[:, kc, mm_ * 128:(mm_ + 1) * 128], ACTB[:, kc, :]) for kc in range(22)])
                        P.tt("dve", XS[:, m, :], XS[:, m, :], ps, ADD)
            if not last:
                P.dma("act", xres.t((slice(None), slice(None), tok), tt), XS)
            else:
                for s in range(4):
                    yo = YO[s % 2]
                    for q in range(4):
                        ps = PSB[pc % 8]
                        pc += 1
                        for j in range(4):
                            kc = q * 4 + j
                            P.tr(ps[:, j * 128:(j + 1) * 128], XS[:, kc, s * 128:(s + 1) * 128], CST[:, 0, :])
                        P.copy("act" if q % 2 == 0 else "dve", yo[:, q * 512:(q + 1) * 512], ps)
                    r0 = tt * 512 + s * 128
                    P.dma("act", y_out.t((slice(r0, r0 + 128), slice(None)), tt), yo)

    order = ["setup", "convert", "params", "p1", "p2a", "p2b", "p2c", "p3"]
    lim = order.index(stop) if stop else len(order)

    def prog():
        setup_x()
        if lim < 1:
            return
        convert(0)
        P.barrier()
        for l in range(depth):
            if lim < 2:
                return
            load_params(l)
            P.barrier()
            if l + 1 < depth:
                convert(l + 1)
            if lim < 3:
                return
            phase1(l)
            P.barrier()
            if lim < 4:
                return
            phase2(l)
            P.barrier()
            if lim < 7:
                return
            phase3(l, l == depth - 1)
            P.barrier()
    prog()
    build.nsems = len(P.dma_sems)
    P.emit()
    return nc


def _consts():
    i = np.arange(128)
    c = np.zeros((128, 4, 128), np.float32)
    c[:, 0, :] = np.eye(128, dtype=np.float32)
    c[:, 1, :] = (i[:, None] <= i[None, :])
    c[:, 2, :] = (i[:, None] > i[None, :])
    c[:, 3, :] = (i[:, None] >= i[None, :])
    return c


def prep_shared(inp, depth):
    f = np.float32
    idx, mask = _bias_index_tables()
    rb = np.asarray(inp["rel_bias"], f)
    ab = np.zeros((12, 2, 128, 128), f)
    for hd in range(12):
        ab[hd] = rb[idx[hd], hd]
    sh = {}
    sh["abias"] = np.ascontiguousarray(ab.transpose(2, 0, 1, 3))
    sh["amask"] = np.ascontiguousarray(mask.transpose(1, 0, 2))
    sh["cst"] = _consts()
    sh["g1"] = np.ascontiguousarray(np.asarray(inp["norm1_g"], f)[:depth].reshape(depth, 16, 128).transpose(0, 2, 1))
    sh["g2"] = np.ascontiguousarray(np.asarray(inp["norm2_g"], f)[:depth].reshape(depth, 16, 128).transpose(0, 2, 1))
    sh["qkg"] = np.ascontiguousarray(np.stack([np.asarray(inp["q_norm_g"], f)[:depth],
                                               np.asarray(inp["k_norm_g"], f)[:depth]], axis=-1))
    sh["gup"] = np.ascontiguousarray(np.asarray(inp["gla_gate_up"], f)[:depth])
    sh["gb"] = np.ascontiguousarray(np.broadcast_to(np.asarray(inp["gla_gate_b"], f)[:depth, None, :], (depth, 128, 384)))
    sh["gog"] = np.ascontiguousarray(np.asarray(inp["gla_out_g"], f)[:depth].reshape(depth, 2, 96).transpose(0, 2, 1))
    sh["lng"] = np.ascontiguousarray(np.broadcast_to(np.asarray(inp["sgu_ln_g"], f)[:depth, None, :], (depth, 128, 768)))
    sh["lnb"] = np.ascontiguousarray(np.broadcast_to(np.asarray(inp["sgu_ln_b"], f)[:depth, None, :], (depth, 128, 768)))
    sh["sw"] = np.ascontiguousarray(np.asarray(inp["sgu_w"], f)[:depth])
    sh["sbias"] = np.ascontiguousarray(np.asarray(inp["sgu_b"], f)[:depth].reshape(depth, 1, 512))
    sh["w_in"] = np.ascontiguousarray(np.asarray(inp["w_in"], f)[:depth])
    sh["w_branch"] = np.ascontiguousarray(np.asarray(inp["w_branch"], f)[:depth])
    sh["w_out"] = np.ascontiguousarray(np.asarray(inp["w_out"], f)[:depth])
    sh["w_ffn_in"] = np.ascontiguousarray(np.asarray(inp["w_ffn_in"], f)[:depth])
    sh["w_ffn_out"] = np.ascontiguousarray(np.asarray(inp["w_ffn_out"], f)[:depth])
    return sh


_NC_CACHE = {}


def run(inp, depth=4, n_cores=8, dbg=(), trace=False, stop=None):
    x = np.asarray(inp["x"], np.float32)
    B, S, _ = x.shape
    assert B == n_cores
    key = (S, depth, tuple(dbg), stop)
    if key not in _NC_CACHE:
        _NC_CACHE[key] = build(S=S, depth=depth, dbg=dbg, stop=stop)
    nc = _NC_CACHE[key]
    sh = prep_shared(inp, depth)
    in_maps = []
    for c in range(n_cores):
        m = dict(sh)
        m["x"] = np.ascontiguousarray(x[c])
        in_maps.append(m)
    res = run_bass_kernel_spmd(nc, in_maps, core_ids=list(range(n_cores)), trace=trace)
    return res


def kernel(**inputs):
    res = run(inputs, depth=4, n_cores=8)
    return np.stack([r["y"] for r in res.results], axis=0).astype(np.float32)
```
